# Optimizing a Trainium2 kernel written in Bass

```python
import math
import jax
import jax.numpy as jnp
from jax import lax
import numpy as np

D_MODEL = 4096
BATCH = 2
SEQ = 8192
DEPTH = 2

MEM_TOKENS = 256
MIX_WIDTH = D_MODEL // 4
N_BRANCH = 3
MLSTM_HEADS = 4
MLSTM_DV = MIX_WIDTH // MLSTM_HEADS
MLSTM_DK = MLSTM_DV // 2
GLA_HEADS = 4
GLA_DV = MIX_WIDTH // GLA_HEADS
GLA_DK = GLA_DV // 2
GLA_RANK = 16
GLA_TAU = 16.0
HYENA_CH = MIX_WIDTH
HYENA_ORDER = 2
HYENA_SHORT = 3
HYENA_BANDS = 16
HYENA_EMB = 1 + 2 * HYENA_BANDS
HYENA_HID = 64
HYENA_MIN_DECAY = math.log(1e-2) / 1.5
HYENA_MAX_DECAY = math.log(1e-2) / 0.3
HYENA_FILTER_SCALE = 0.005
CHUNK = 64
XATTN_HEADS = 4
XATTN_HD = D_MODEL // XATTN_HEADS
D_FF = 4 * D_MODEL
EPS = 1e-6

SEG_SIZES = (
    MLSTM_HEADS * MLSTM_DK,
    MLSTM_HEADS * MLSTM_DK,
    MIX_WIDTH,
    MIX_WIDTH,
    4 * MLSTM_HEADS,
    GLA_HEADS * GLA_DK,
    GLA_HEADS * GLA_DK,
    MIX_WIDTH,
    MIX_WIDTH,
    2 * GLA_RANK,
    3 * HYENA_CH,
    N_BRANCH * D_MODEL,
)
SEG_OFFSETS = tuple(sum(SEG_SIZES[:i + 1]) for i in range(len(SEG_SIZES) - 1))
IN_WIDTH = sum(SEG_SIZES)
MLSTM_GATE_START = SEG_OFFSETS[3]

kernel_name = 'hybrid_mlstm_gla_hyena_encoder'


def rmsnorm(x, g):
    xf = x.astype(jnp.float32)
    r = lax.rsqrt(jnp.mean(xf * xf, axis=-1, keepdims=True) + EPS)
    return (xf * r).astype(x.dtype) * g


def head_rmsnorm(h, g):
    B, L, H, d = h.shape
    return rmsnorm(h, g.reshape(H, d)).reshape(B, L, H * d)


def flip(a):
    return jnp.flip(a, axis=1)


def to_chunks(a):
    B, L, H, d = a.shape
    return a.reshape(B, L // CHUNK, CHUNK, H, d).transpose(1, 0, 3, 2, 4)


def from_chunks(a):
    NC, B, H, C, d = a.shape
    return a.transpose(1, 0, 3, 2, 4).reshape(B, NC * C, H, d)


def mlstm_scan(q, k, v, i_pre, f_pre):
    B, L, H, dk = q.shape
    dv = v.shape[-1]
    qc, kc, vc = to_chunks(q), to_chunks(k), to_chunks(v)
    ic = to_chunks(i_pre[..., None])[..., 0]
    lfc = to_chunks(jax.nn.log_sigmoid(f_pre)[..., None])[..., 0]
    tri = jnp.tril(jnp.ones((CHUNK, CHUNK), dtype=bool))

    def step(carry, xs):
        C, n, m = carry
        qj, ks, vs, ig, lf = xs
        b = jnp.cumsum(lf, axis=-1)
        g = b[..., -1]
        logD = jnp.where(tri, b[..., :, None] - b[..., None, :] + ig[..., None, :], -jnp.inf)
        m_inter = b + m[..., None]
        m_j = jnp.maximum(m_inter, jnp.max(logD, axis=-1))
        s = jnp.einsum('bhjd,bhsd->bhjs', qj, ks) * jnp.exp(logD - m_j[..., None])
        w_inter = jnp.exp(m_inter - m_j)
        num = jnp.einsum('bhjs,bhsv->bhjv', s, vs) + w_inter[..., None] * jnp.einsum('bhjd,bhdv->bhjv', qj, C)
        den = jnp.sum(s, axis=-1) + w_inter * jnp.einsum('bhjd,bhd->bhj', qj, n)
        h = num / jnp.maximum(jnp.abs(den), jnp.exp(-m_j))[..., None]
        a = g[..., None] - b + ig
        m_new = jnp.maximum(g + m, jnp.max(a, axis=-1))
        wk = jnp.exp(a - m_new[..., None])
        decay = jnp.exp(g + m - m_new)
        C = decay[..., None, None] * C + jnp.einsum('bhs,bhsd,bhsv->bhdv', wk, ks, vs)
        n = decay[..., None] * n + jnp.einsum('bhs,bhsd->bhd', wk, ks)
        return (C, n, m_new), h

    init = (jnp.zeros((B, H, dk, dv), jnp.float32),
            jnp.zeros((B, H, dk), jnp.float32),
            jnp.zeros((B, H), jnp.float32))
    _, h = lax.scan(step, init, (qc, kc, vc, ic, lfc))
    return from_chunks(h)


def gla_scan(q, k, v, log_a):
    B, L, H, dk = q.shape
    dv = v.shape[-1]
    qc, kc, vc, ac = to_chunks(q), to_chunks(k), to_chunks(v), to_chunks(log_a)
    tri = jnp.tril(jnp.ones((CHUNK, CHUNK), dtype=bool))[:, :, None]

    def step(S, xs):
        qj, ks, vs, la = xs
        b = jnp.cumsum(la, axis=-2)
        rel = jnp.where(tri, b[..., :, None, :] - b[..., None, :, :], -jnp.inf)
        att = jnp.einsum('bhjd,bhsd,bhjsd->bhjs', qj, ks, jnp.exp(rel))
        o = jnp.einsum('bhjs,bhsv->bhjv', att, vs) + jnp.einsum('bhjd,bhdv->bhjv', qj * jnp.exp(b), S)
        g = b[..., -1, :]
        S = jnp.exp(g)[..., None] * S + jnp.einsum('bhsd,bhsv->bhdv', ks * jnp.exp(g[..., None, :] - b), vs)
        return S, o

    _, o = lax.scan(step, jnp.zeros((B, H, dk, dv), jnp.float32), (qc, kc, vc, ac))
    return from_chunks(o)


def mlstm_branch(q_in, k_in, v_in, o_in, gate_in, head_norm):
    B, L, _ = q_in.shape
    H = MLSTM_HEADS
    f32 = jnp.float32
    q = q_in.astype(f32).reshape(B, L, H, MLSTM_DK) * MLSTM_DK ** -0.5
    k = k_in.astype(f32).reshape(B, L, H, MLSTM_DK)
    v = v_in.astype(f32).reshape(B, L, H, MLSTM_DV)
    gts = gate_in.astype(f32).reshape(B, L, 4, H)
    h_fwd = mlstm_scan(q, k, v, gts[:, :, 0], gts[:, :, 1])
    h_bwd = flip(mlstm_scan(flip(q), flip(k), flip(v), flip(gts[:, :, 2]), flip(gts[:, :, 3])))
    h = head_rmsnorm(h_fwd + h_bwd, head_norm).astype(o_in.dtype)
    return h * jax.nn.sigmoid(o_in)


def gla_branch(q_in, k_in, v_in, g_in, lr_in, decay_up, decay_bias, head_norm):
    B, L, _ = q_in.shape
    H = GLA_HEADS
    f32 = jnp.float32
    q = q_in.astype(f32).reshape(B, L, H, GLA_DK) * GLA_DK ** -0.5
    k = k_in.astype(f32).reshape(B, L, H, GLA_DK)
    v = v_in.astype(f32).reshape(B, L, H, GLA_DV)
    lr = lr_in.reshape(B, L, 2, GLA_RANK)
    z = (jnp.einsum('blzr,zrk->blzk', lr, decay_up) + decay_bias).astype(f32)
    log_a = jax.nn.log_sigmoid(z) / GLA_TAU
    la_fwd = log_a[:, :, 0].reshape(B, L, H, GLA_DK)
    la_bwd = log_a[:, :, 1].reshape(B, L, H, GLA_DK)
    o = gla_scan(q, k, v, la_fwd) + flip(gla_scan(flip(q), flip(k), flip(v), flip(la_bwd)))
    o = head_rmsnorm(o, head_norm).astype(g_in.dtype)
    return o * jax.nn.silu(g_in)


def short_conv(u, w, b):
    L = u.shape[1]
    pad = HYENA_SHORT // 2
    up = jnp.pad(u, ((0, 0), (pad, pad), (0, 0)))
    return sum(up[:, j:j + L] * w[j] for j in range(HYENA_SHORT)) + b


def hyena_filters(L, w1, b1, w2, b2, w3, freq):
    f32 = jnp.float32
    n = jnp.arange(L, dtype=f32)
    t = n / (L - 1)
    bands = jnp.linspace(1e-4, HYENA_BANDS - 1, HYENA_BANDS, dtype=f32)
    ang = (2.0 * math.pi * n / L)[:, None] * bands[None, :]
    z = jnp.concatenate([t[:, None], jnp.cos(ang), -jnp.sin(ang)], axis=-1)
    h = jnp.sin(freq * (z @ w1 + b1))
    h = jnp.sin(freq * (h @ w2 + b2))
    h = (h @ w3).astype(f32).reshape(L, HYENA_ORDER, 2, HYENA_CH)
    deltas = jnp.abs(jnp.linspace(HYENA_MIN_DECAY, HYENA_MAX_DECAY, HYENA_CH, dtype=f32))
    window = jnp.exp(-t[:, None] * deltas[None, :])
    return h * window[:, None, None, :]


def fft_conv_bidir(u, h_fwd, h_bwd, skip):
    L = u.shape[1]
    k = jnp.concatenate([h_fwd, jnp.zeros_like(h_fwd[:1]), h_bwd[:0:-1]], axis=0)
    uf = jnp.fft.rfft(u, n=2 * L, axis=1)
    kf = jnp.fft.rfft(k, n=2 * L, axis=0)
    y = jnp.fft.irfft(uf * kf[None], n=2 * L, axis=1)[:, :L]
    return y + skip * u


def hyena_branch(hy_in, conv_w, conv_b, w1, b1, w2, b2, w3, freq, skip):
    L = hy_in.shape[1]
    u = short_conv(hy_in, conv_w, conv_b).astype(jnp.float32)
    v, x1, x2 = jnp.split(u, 3, axis=-1)
    filt = hyena_filters(L, w1, b1, w2, b2, w3, freq)
    skip = skip.astype(jnp.float32)
    z = v
    for o, gate in enumerate((x1, x2)):
        z = gate * fft_conv_bidir(z, filt[:, o, 0], filt[:, o, 1], skip[o])
    return z.astype(hy_in.dtype)


def mixer_block(xn, w_in, b_in, mlstm_head_norm, gla_decay_up, gla_decay_bias, gla_head_norm,
                hyena_conv_w, hyena_conv_b, hyena_ffn_w1, hyena_ffn_b1, hyena_ffn_w2, hyena_ffn_b2,
                hyena_ffn_w3, hyena_freq, hyena_skip, w_branch, w_out):
    B, L, _ = xn.shape
    proj = xn @ w_in + b_in
    (mq, mk, mv, mo, mg, gq, gk, gv, gg, glr, hy, gates) = jnp.split(proj, SEG_OFFSETS, axis=-1)
    y_a = mlstm_branch(mq, mk, mv, mo, mg, mlstm_head_norm)
    y_b = gla_branch(gq, gk, gv, gg, glr, gla_decay_up, gla_decay_bias, gla_head_norm)
    y_c = hyena_branch(hy, hyena_conv_w, hyena_conv_b, hyena_ffn_w1, hyena_ffn_b1, hyena_ffn_w2,
                       hyena_ffn_b2, hyena_ffn_w3, hyena_freq, hyena_skip)
    branches = jnp.stack([y_a, y_b, y_c], axis=2)
    up = jnp.einsum('blnc,ncd->blnd', branches, w_branch)
    gate = jax.nn.sigmoid(gates.reshape(B, L, N_BRANCH, D_MODEL))
    merged = jnp.sum(up * gate, axis=2)
    return merged @ w_out


def cross_attention(xn, memn, wq, wkv, wo):
    B, L, _ = xn.shape
    M = memn.shape[1]
    q = (xn @ wq).reshape(B, L, XATTN_HEADS, XATTN_HD)
    kv = memn @ wkv
    k, v = jnp.split(kv, 2, axis=-1)
    k = k.reshape(B, M, XATTN_HEADS, XATTN_HD)
    v = v.reshape(B, M, XATTN_HEADS, XATTN_HD)
    s = jnp.einsum('blhd,bmhd->bhlm', q, k).astype(jnp.float32) * XATTN_HD ** -0.5
    p = jax.nn.softmax(s, axis=-1).astype(v.dtype)
    o = jnp.einsum('bhlm,bmhd->blhd', p, v).reshape(B, L, D_MODEL)
    return o @ wo


def squared_relu_mlp(xn, w1, w2):
    return jnp.square(jax.nn.relu(xn @ w1)) @ w2


def setup_inputs(seed: int = 0) -> dict:
    key = jax.random.key(seed)
    ks = jax.random.split(key, 32)
    f32 = jnp.float32
    D, H = D_MODEL, MLSTM_HEADS

    def nrm(k, shape, scale):
        return jax.random.normal(k, shape, f32) * scale

    x = nrm(ks[0], (BATCH, SEQ, D), 1.0)
    mem = nrm(ks[1], (BATCH, MEM_TOKENS, D), 1.0)
    norm_gains = 1.0 + nrm(ks[2], (DEPTH, 4, D), 0.02)
    final_norm = 1.0 + nrm(ks[3], (D,), 0.02)
    w_in = nrm(ks[4], (DEPTH, D, IN_WIDTH), D ** -0.5)
    fbias = jnp.linspace(3.0, 6.0, H, dtype=f32)
    g0 = MLSTM_GATE_START
    b_in = nrm(ks[5], (DEPTH, IN_WIDTH), 0.02)
    b_in = b_in.at[:, g0 + H:g0 + 2 * H].add(fbias).at[:, g0 + 3 * H:g0 + 4 * H].add(fbias)
    mlstm_head_norm = 1.0 + nrm(ks[6], (DEPTH, MIX_WIDTH), 0.02)
    gla_decay_up = nrm(ks[7], (DEPTH, 2, GLA_RANK, GLA_HEADS * GLA_DK), GLA_RANK ** -0.5)
    gla_decay_bias = nrm(ks[8], (DEPTH, 2, GLA_HEADS * GLA_DK), 0.1)
    gla_head_norm = 1.0 + nrm(ks[9], (DEPTH, MIX_WIDTH), 0.02)
    hyena_conv_w = nrm(ks[10], (DEPTH, HYENA_SHORT, 3 * HYENA_CH), HYENA_SHORT ** -0.5)
    hyena_conv_b = nrm(ks[11], (DEPTH, 3 * HYENA_CH), 0.02)
    hyena_ffn_w1 = nrm(ks[12], (DEPTH, HYENA_EMB, HYENA_HID), HYENA_EMB ** -0.5)
    hyena_ffn_b1 = nrm(ks[13], (DEPTH, HYENA_HID), 0.1)
    hyena_ffn_w2 = nrm(ks[14], (DEPTH, HYENA_HID, HYENA_HID), HYENA_HID ** -0.5)
    hyena_ffn_b2 = nrm(ks[15], (DEPTH, HYENA_HID), 0.1)
    hyena_ffn_w3 = nrm(ks[16], (DEPTH, HYENA_HID, HYENA_ORDER * 2 * HYENA_CH), HYENA_FILTER_SCALE)
    hyena_freq = 1.0 + nrm(ks[17], (DEPTH, HYENA_HID), 0.02)
    hyena_skip = nrm(ks[18], (DEPTH, HYENA_ORDER, HYENA_CH), 1.0)
    w_branch = nrm(ks[19], (DEPTH, N_BRANCH, MIX_WIDTH, D), MIX_WIDTH ** -0.5)
    w_out = nrm(ks[20], (DEPTH, D, D), D ** -0.5)
    xattn_wq = nrm(ks[21], (DEPTH, D, D), D ** -0.5)
    xattn_wkv = nrm(ks[22], (DEPTH, D, 2 * D), D ** -0.5)
    xattn_wo = nrm(ks[23], (DEPTH, D, D), D ** -0.5)
    mlp_w1 = nrm(ks[24], (DEPTH, D, D_FF), D ** -0.5)
    mlp_w2 = nrm(ks[25], (DEPTH, D_FF, D), D_FF ** -0.5)
    return {'x': x, 'mem': mem, 'norm_gains': norm_gains, 'final_norm': final_norm,
            'w_in': w_in, 'b_in': b_in, 'mlstm_head_norm': mlstm_head_norm,
            'gla_decay_up': gla_decay_up, 'gla_decay_bias': gla_decay_bias, 'gla_head_norm': gla_head_norm,
            'hyena_conv_w': hyena_conv_w, 'hyena_conv_b': hyena_conv_b,
            'hyena_ffn_w1': hyena_ffn_w1, 'hyena_ffn_b1': hyena_ffn_b1,
            'hyena_ffn_w2': hyena_ffn_w2, 'hyena_ffn_b2': hyena_ffn_b2,
            'hyena_ffn_w3': hyena_ffn_w3, 'hyena_freq': hyena_freq, 'hyena_skip': hyena_skip,
            'w_branch': w_branch, 'w_out': w_out,
            'xattn_wq': xattn_wq, 'xattn_wkv': xattn_wkv, 'xattn_wo': xattn_wo,
            'mlp_w1': mlp_w1, 'mlp_w2': mlp_w2}


def reference(x, mem, norm_gains, final_norm, w_in, b_in, mlstm_head_norm,
              gla_decay_up, gla_decay_bias, gla_head_norm,
              hyena_conv_w, hyena_conv_b, hyena_ffn_w1, hyena_ffn_b1,
              hyena_ffn_w2, hyena_ffn_b2, hyena_ffn_w3, hyena_freq, hyena_skip,
              w_branch, w_out, xattn_wq, xattn_wkv, xattn_wo, mlp_w1, mlp_w2):
    h = x
    for l in range(DEPTH):
        g = norm_gains[l]
        h = h + mixer_block(rmsnorm(h, g[0]), w_in[l], b_in[l], mlstm_head_norm[l],
                            gla_decay_up[l], gla_decay_bias[l], gla_head_norm[l],
                            hyena_conv_w[l], hyena_conv_b[l], hyena_ffn_w1[l], hyena_ffn_b1[l],
                            hyena_ffn_w2[l], hyena_ffn_b2[l], hyena_ffn_w3[l], hyena_freq[l],
                            hyena_skip[l], w_branch[l], w_out[l])
        h = h + cross_attention(rmsnorm(h, g[1]), rmsnorm(mem, g[2]),
                                xattn_wq[l], xattn_wkv[l], xattn_wo[l])
        h = h + squared_relu_mlp(rmsnorm(h, g[3]), mlp_w1[l], mlp_w2[l])
    return rmsnorm(h, final_norm)
```

```python
import contextlib
import numpy as np
import ml_dtypes
import concourse.bass as bass
import concourse.mybir as mybir
from concourse.bass_utils import run_bass_kernel_spmd

F32 = mybir.dt.float32
BF16 = mybir.dt.bfloat16
ALU = mybir.AluOpType
AF = mybir.ActivationFunctionType
AX = mybir.AxisListType

EPOCH = 20000
NDSEM = 6


class Buf:
    def __init__(self, name, ap=None):
        self.name = name
        self.ap = ap
        self.w = {}
        self.r = {}
        self.wold = {}

    def __getitem__(self, k):
        return self.ap[k]


class Ctx:
    def __init__(self, nc, es):
        self.nc = nc
        self.es = es
        self.eng = {"pe": nc.tensor, "dve": nc.vector, "act": nc.scalar, "pool": nc.gpsimd, "sp": nc.sync}
        self.cnt = {k: 0 for k in self.eng}
        self.sems = {}
        self.waited = {k: {} for k in self.eng}
        self.dcnt = {k: 0 for k in self.eng}
        self.nsb = 0

    def _sem(self, key):
        if key not in self.sems:
            self.sems[key] = self.es.enter_context(self.nc.semaphore("s_%s_%s" % key))
        return self.sems[key]

    def _wait(self, e, key, val):
        if self.waited[e].get(key, 0) >= val:
            return
        self.eng[e].wait_ge(self._sem(key), val)
        self.waited[e][key] = val

    def _deps(self, e, outs, ins, acc=False, part=False):
        for b in ins:
            for k, v in b.w.items():
                self._wait(e, k, v)
        for b in outs:
            for k, v in b.r.items():
                self._wait(e, k, v)
            for k, v in b.wold.items():
                self._wait(e, k, v)
            if part:
                continue
            for k, v in b.w.items():
                if acc and k[0] == e:
                    continue
                self._wait(e, k, v)

    def _commit(self, tok, outs, ins, part=False):
        k, v = tok
        for b in ins:
            b.r[k] = max(b.r.get(k, 0), v)
        for b in outs:
            if part:
                b.w[k] = max(b.w.get(k, 0), v)
            else:
                for kk, vv in b.w.items():
                    b.wold[kk] = max(b.wold.get(kk, 0), vv)
                b.w.clear()
                b.w[k] = v

    def op(self, e, fn, outs, ins, acc=False, part=False):
        self._deps(e, outs, ins, acc, part)
        inst = fn()
        n = self.cnt[e]
        key = (e, n // EPOCH)
        inst.then_inc(self._sem(key), 1)
        self.cnt[e] = n + 1
        tok = (key, n % EPOCH + 1)
        self._commit(tok, outs, ins, part or acc)
        return tok

    def dma(self, q, out, in_, outs, ins, part=False, **kw):
        d = self.dcnt[q]
        key = ("d" + q, d % NDSEM)
        if d >= NDSEM:
            self._wait(q, key, 16 * (d // NDSEM))
        self._deps(q, outs, ins, False, part)
        inst = self.eng[q].dma_start(out=out, in_=in_, **kw)
        inst.then_inc(self._sem(key), 16)
        self.dcnt[q] = d + 1
        tok = (key, 16 * (d // NDSEM + 1))
        self._commit(tok, outs, ins, part)
        return tok

    def barrier(self, engines=None):
        toks = []
        for e, n in self.cnt.items():
            if n > 0:
                toks.append(((e, (n - 1) // EPOCH), (n - 1) % EPOCH + 1))
        for q, d in self.dcnt.items():
            for i in range(min(d, NDSEM)):
                uses = (d - 1 - i) // NDSEM + 1
                toks.append((("d" + q, i), 16 * uses))
        for e in (engines or self.eng):
            for k, v in toks:
                self._wait(e, k, v)

    def sb(self, shape, dt, name=None, es=None):
        self.nsb += 1
        name = name or "t%d" % self.nsb
        t = (es or self.es).enter_context(self.nc.sbuf_tensor(name + "_%d" % self.nsb, list(shape), dt))
        return Buf(name, t)

    def ps(self, shape, dt=F32, name=None, es=None):
        self.nsb += 1
        name = name or "p%d" % self.nsb
        t = (es or self.es).enter_context(self.nc.psum_tensor(name + "_%d" % self.nsb, list(shape), dt))
        return Buf(name, t)

    def dram(self, shape, dt, name):
        if name in getattr(self, "dbg", ()):
            t = self.nc.dram_tensor(name, list(shape), dt, kind="ExternalOutput")
        else:
            t = self.nc.dram_tensor(name, list(shape), dt)
        return Buf(name, t.ap())

    def finish(self, bufs):
        for b in bufs:
            for k, v in b.w.items():
                self._wait("sp", k, v)


D = 4096
EPS = 1e-6
WQ_W = ("sp", "act")


class Rot:
    def __init__(self, bufs):
        self.bufs = bufs; self.i = 0
    def next(self):
        b = self.bufs[self.i % len(self.bufs)]; self.i += 1
        return b


def cast_copy(ctx, e, out_ap, in_ap, outs, ins, part=False):
    nc = ctx.nc
    if e == "act":
        return ctx.op("act", lambda: nc.scalar.copy(out=out_ap, in_=in_ap), outs=outs, ins=ins, part=part)
    return ctx.op(e, lambda: ctx.eng[e].tensor_copy(out=out_ap, in_=in_ap), outs=outs, ins=ins, part=part)


def gemm_fm(ctx, groups, out, N, T, epi, TT=1024, wq=("sp", "pool"), cast_engs=("dve", "act"), out_dt=None, n_ot=2):
    nc = ctx.nc
    ctx.barrier()
    NB = (N + 127) // 128
    TT = min(TT, T)
    NT = T // TT
    SW = min(512, TT)
    NS = TT // SW
    out_dt = out_dt or out.ap.dtype
    KCs = [g[3] // 128 for g in groups]
    KCT = sum(KCs)
    with contextlib.ExitStack() as es:
        xt = ctx.sb([128, KCT, TT], BF16, "xt", es)
        wf = Rot([ctx.sb([128, max(KCs), 128], F32, "wf", es) for _ in range(max(3, len(groups) + 1))])
        wb = Rot([ctx.sb([128, max(KCs), 128], BF16, "wb", es) for _ in range(len(groups) + 2)])
        ot = Rot([ctx.sb([128, TT], out_dt, "ot", es) for _ in range(n_ot)])
        pss = Rot([ctx.ps([128, 512], F32, "ps", es) for _ in range(4 if len(groups) == 1 else 6)])
        it = 0
        for tt in range(NT):
            k0 = 0
            first = True
            for (W, X, xr0, K), KC in zip(groups, KCs):
                Xv = X.ap[xr0:xr0 + K, :].rearrange("(kc p) t -> p kc t", p=128)
                nsplit = min(4, KC)
                step = KC // nsplit
                for s in range(nsplit):
                    ctx.dma(wq[s % len(wq)], xt.ap[:, k0 + s * step:k0 + (s + 1) * step, :],
                            Xv[:, s * step:(s + 1) * step, tt * TT:(tt + 1) * TT], outs=[xt], ins=[X], part=not first)
                    first = False
                k0 += KC
            for nb in range(NB):
                n0 = nb * 128
                nn = min(128, N - n0)
                o = ot.next()
                wbs = []
                for (W, X, xr0, K), KC in zip(groups, KCs):
                    Wv = W.rearrange("(kc p) n -> p kc n", p=128)
                    f = wf.next(); b = wb.next()
                    ctx.dma(WQ_W[it % len(WQ_W)], f.ap[:, :KC, :nn], Wv[:, :, n0:n0 + nn], outs=[f], ins=[])
                    cast_copy(ctx, cast_engs[it % len(cast_engs)], b.ap[:, :KC, :nn], f.ap[:, :KC, :nn], [b], [f])
                    wbs.append(b)
                    it += 1
                for s in range(NS):
                    pl = []
                    k0 = 0
                    for b, KC in zip(wbs, KCs):
                        ps = pss.next()
                        for kc in range(KC):
                            ctx.op("pe", lambda: nc.tensor.matmul(ps.ap[:nn, :SW], lhsT=b.ap[:, kc, :nn],
                                                                     rhs=xt.ap[:, k0 + kc, s * SW:(s + 1) * SW],
                                                                     start=(kc == 0), stop=(kc == KC - 1)),
                                   outs=[ps], ins=[b, xt], acc=True)
                        k0 += KC
                        pl.append(ps)
                    epi(ctx, pl, o, nb, nn, tt * TT + s * SW, s, SW)
                ctx.dma(wq[it % len(wq)], out.ap[n0:n0 + nn, tt * TT:(tt + 1) * TT], o.ap[:nn, :], outs=[out], ins=[o], part=True)


def gemm_tm(ctx, W, X, out, K, N, T, bias_bc=None, CG=512, wq=("sp", "pool"), out_dt=None):
    nc = ctx.nc
    ctx.barrier()
    KC = K // 128
    out_dt = out_dt or out.ap.dtype
    NG = (N + CG - 1) // CG
    TB = min(512, T)
    with contextlib.ExitStack() as es:
        wb = ctx.sb([128, KC, CG], BF16, "twb", es)
        wf = Rot([ctx.sb([128, 8, CG], F32, "twf", es) for _ in range(2)])
        xt = Rot([ctx.sb([128, KC, TB], BF16, "txt", es) for _ in range(2)])
        ot = Rot([ctx.sb([128, CG], out_dt, "tot", es) for _ in range(2)])
        pss = Rot([ctx.ps([128, 512], F32, "tps", es) for _ in range(2)])
        Wv = W.rearrange("(kc p) n -> p kc n", p=128)
        Xv = X.ap.rearrange("(kc p) t -> p kc t", p=128)
        it = 0
        for g in range(NG):
            c0 = g * CG
            cn = min(CG, N - c0)
            for k8 in range(0, KC, 8):
                f = wf.next()
                kn = min(8, KC - k8)
                ctx.dma(wq[it % 2], f.ap[:, :kn, :cn], Wv[:, k8:k8 + kn, c0:c0 + cn], outs=[f], ins=[])
                cast_copy(ctx, ("dve", "act")[it % 2], wb.ap[:, k8:k8 + kn, :cn], f.ap[:, :kn, :cn], [wb], [f], part=(k8 > 0))
                it += 1
            for tb in range(T // TB):
                x = xt.next()
                for s in range(2):
                    h = KC // 2
                    ctx.dma(wq[s], x.ap[:, s * h:(s + 1) * h, :], Xv[:, s * h:(s + 1) * h, tb * TB:(tb + 1) * TB], outs=[x], ins=[X], part=(s > 0))
                for t4 in range(TB // 128):
                    ps = pss.next()
                    for kc in range(KC):
                        ctx.op("pe", lambda: nc.tensor.matmul(ps.ap[:, :cn], lhsT=x.ap[:, kc, t4 * 128:(t4 + 1) * 128], rhs=wb.ap[:, kc, :cn],
                                                                 start=(kc == 0), stop=(kc == KC - 1)), outs=[ps], ins=[x, wb], acc=True)
                    o = ot.next()
                    if bias_bc is not None:
                        ctx.op("dve", lambda: nc.vector.tensor_tensor(out=o.ap[:, :cn], in0=ps.ap[:, :cn], in1=bias_bc.ap[:, c0:c0 + cn], op=ALU.add),
                               outs=[o], ins=[ps, bias_bc])
                    else:
                        cast_copy(ctx, "dve", o.ap[:, :cn], ps.ap[:, :cn], [o], [ps])
                    t0 = tb * TB + t4 * 128
                    ctx.dma(wq[it % 2], out.ap[t0:t0 + 128, c0:c0 + cn], o.ap[:, :cn], outs=[out], ins=[o], part=True)
                    it += 1


def norm_fm(ctx, H, gain, out, T, consts, TT=256, Drows=D, wq=("sp", "pool")):
    nc = ctx.nc
    ctx.barrier()
    KC = Drows // 128
    TT = min(TT, T)
    ones = consts["ones_bf"]; eps = consts["eps"]
    with contextlib.ExitStack() as es:
        hs = Rot([ctx.sb([128, KC, TT], F32, "nh", es) for _ in range(2)])
        sq = ctx.sb([128, KC, TT], BF16, "nsq", es)
        xo = Rot([ctx.sb([128, KC, TT], out.ap.dtype, "nx", es) for _ in range(2)])
        R = ctx.sb([128, TT], F32, "nR", es)
        ps = ctx.ps([128, 512], F32, "nps", es)
        Hv = H.ap.rearrange("(kc p) t -> p kc t", p=128)
        Ov = out.ap.rearrange("(kc p) t -> p kc t", p=128)
        for tt in range(T // TT):
            h = hs.next()
            hh = KC // 2
            for s in range(2):
                ctx.dma(wq[s], h.ap[:, s * hh:(s + 1) * hh, :], Hv[:, s * hh:(s + 1) * hh, tt * TT:(tt + 1) * TT], outs=[h], ins=[H], part=(s > 0))
            ctx.op("act", lambda: nc.scalar.activation(out=sq.ap[:], in_=h.ap[:], func=AF.Square), outs=[sq], ins=[h])
            for kc in range(KC):
                ctx.op("pe", lambda: nc.tensor.matmul(ps.ap[:, :TT], lhsT=ones.ap[:, :], rhs=sq.ap[:, kc, :], start=(kc == 0), stop=(kc == KC - 1)),
                       outs=[ps], ins=[ones, sq], acc=True)
            ctx.op("act", lambda: nc.scalar.activation(out=R.ap[:], in_=ps.ap[:, :TT], func=AF.Sqrt, bias=eps.ap[:, 0:1], scale=1.0 / Drows), outs=[R], ins=[ps, eps])
            ctx.op("dve", lambda: nc.vector.reciprocal(out=R.ap[:], in_=R.ap[:]), outs=[R], ins=[R])
            x = xo.next()
            for kc in range(KC):
                e = "dve"
                ctx.op(e, lambda: ctx.eng[e].scalar_tensor_tensor(out=x.ap[:, kc, :], in0=h.ap[:, kc, :], scalar=gain.ap[:, kc:kc + 1], in1=R.ap[:],
                                                                  op0=ALU.mult, op1=ALU.mult), outs=[x], ins=[h, gain, R], part=(kc > 0))
            for s in range(2):
                ctx.dma(wq[s], Ov[:, s * hh:(s + 1) * hh, tt * TT:(tt + 1) * TT], x.ap[:, s * hh:(s + 1) * hh, :], outs=[out], ins=[x], part=True)


def load_const(ctx, ap_dram, shape, dt, name, q="sp"):
    t = ctx.sb(shape, dt, name)
    ctx.dma(q, t.ap[:], ap_dram, outs=[t], ins=[])
    return t


def make_consts(ctx, ins):
    nc = ctx.nc
    c = {}
    c["ones_f"] = ctx.sb([128, 128], F32, "ones_f")
    ctx.op("pool", lambda: nc.gpsimd.memset(c["ones_f"].ap[:], 1.0), outs=[c["ones_f"]], ins=[])
    c["ones_bf"] = ctx.sb([128, 128], BF16, "ones_bf")
    ctx.op("pool", lambda: nc.gpsimd.memset(c["ones_bf"].ap[:], 1.0), outs=[c["ones_bf"]], ins=[])
    c["eps"] = ctx.sb([128, 1], F32, "eps")
    ctx.op("pool", lambda: nc.gpsimd.memset(c["eps"].ap[:], EPS), outs=[c["eps"]], ins=[])
    return c


def attention(ctx, qT, kT, vtok, oT, T, consts, NH=4, HD=1024, M=256, wq=("sp", "pool")):
    nc = ctx.nc
    ctx.barrier()
    HC = HD // 128
    MC = M // 128
    TT = min(512, T)
    ones = consts["ones_bf"]
    with contextlib.ExitStack() as es:
        kt = ctx.sb([128, HC, M], BF16, "akt", es)
        vt = ctx.sb([128, MC, HD], BF16, "avt", es)
        qt = Rot([ctx.sb([128, HC, TT], BF16, "aqt", es) for _ in range(2)])
        et = Rot([ctx.sb([128, MC, TT], BF16, "aet", es) for _ in range(2)])
        rd = Rot([ctx.sb([128, TT], F32, "ard", es) for _ in range(2)])
        ot = Rot([ctx.sb([128, HC, TT], BF16, "aot", es) for _ in range(2)])
        pss = Rot([ctx.ps([128, 512], F32, "aps", es) for _ in range(4)])
        psd = ctx.ps([128, 512], F32, "apd", es)
        for hd in range(NH):
            r0 = hd * HD
            ctx.dma("sp", kt.ap[:], kT.ap[r0:r0 + HD, :].rearrange("(c p) m -> p c m", p=128), outs=[kt], ins=[kT])
            ctx.dma("pool", vt.ap[:], vtok.ap[:, r0:r0 + HD].rearrange("(c p) d -> p c d", p=128), outs=[vt], ins=[vtok])
            for tt in range(T // TT):
                q = qt.next()
                ctx.dma(wq[tt % 2], q.ap[:], qT.ap[r0:r0 + HD, tt * TT:(tt + 1) * TT].rearrange("(c p) t -> p c t", p=128), outs=[q], ins=[qT])
                e = et.next()
                for mc in range(MC):
                    ps = pss.next()
                    for c in range(HC):
                        ctx.op("pe", lambda: nc.tensor.matmul(ps.ap[:, :TT], lhsT=kt.ap[:, c, mc * 128:(mc + 1) * 128], rhs=q.ap[:, c, :],
                                                                 start=(c == 0), stop=(c == HC - 1)), outs=[ps], ins=[kt, q], acc=True)
                    ctx.op("act", lambda: nc.scalar.activation(out=e.ap[:, mc, :], in_=ps.ap[:, :TT], func=AF.Exp, scale=HD ** -0.5),
                           outs=[e], ins=[ps], part=(mc > 0))
                for mc in range(MC):
                    ctx.op("pe", lambda: nc.tensor.matmul(psd.ap[:, :TT], lhsT=ones.ap[:, :], rhs=e.ap[:, mc, :], start=(mc == 0), stop=(mc == MC - 1)),
                           outs=[psd], ins=[ones, e], acc=True)
                r = rd.next()
                ctx.op("dve", lambda: nc.vector.reciprocal(out=r.ap[:], in_=psd.ap[:, :TT]), outs=[r], ins=[psd])
                o = ot.next()
                for c in range(HC):
                    ps = pss.next()
                    for mc in range(MC):
                        ctx.op("pe", lambda: nc.tensor.matmul(ps.ap[:, :TT], lhsT=vt.ap[:, mc, c * 128:(c + 1) * 128], rhs=e.ap[:, mc, :],
                                                                 start=(mc == 0), stop=(mc == MC - 1)), outs=[ps], ins=[vt, e], acc=True)
                    ctx.op("dve", lambda: nc.vector.tensor_tensor(out=o.ap[:, c, :], in0=ps.ap[:, :TT], in1=r.ap[:], op=ALU.mult),
                           outs=[o], ins=[ps, r], part=(c > 0))
                ctx.dma(wq[(tt + 1) % 2], oT.ap[r0:r0 + HD, tt * TT:(tt + 1) * TT].rearrange("(c p) t -> p c t", p=128), o.ap[:], outs=[oT], ins=[o], part=True)


def build_B(T, last, M=256, DFF=4 * D, dbg=()):
    nc = bass.Bass("TRN2", target_bir_lowering=False)
    def din(name, shape, dt=F32):
        return nc.dram_tensor(name, list(shape), dt, kind="ExternalInput").ap()
    hT = Buf("hT", din("hT", [D, T]))
    yT = Buf("yT", din("yT", [3 * 1024, T], BF16))
    memT = Buf("memT", din("memT", [D, M]))
    gains = din("gains", [128, 5 * 32])
    Wg = din("Wg", [D, 3 * D]); bg = din("bg", [128, 96])
    Wbr = din("Wbr", [3, 1024, D]); Wout = din("Wout", [D, D]); Wq = din("Wq", [D, D])
    Wkv = din("Wkv", [D, 2 * D]); Wo = din("Wo", [D, D]); W1 = din("W1", [D, DFF]); W2 = din("W2", [DFF, D])
    outd = nc.dram_tensor("out", [D, T], F32, kind="ExternalOutput")
    out = Buf("out", outd.ap())
    with contextlib.ExitStack() as es:
        ctx = Ctx(nc, es)
        ctx.dbg = dbg
        C = make_consts(ctx, None)
        gt = load_const(ctx, gains, [128, 160], F32, "gains")
        bgt = load_const(ctx, bg, [128, 96], F32, "bg", q="pool")
        def gain(i):
            b = Buf("g%d" % i, gt.ap[:, i * 32:(i + 1) * 32]); b.w = gt.w
            return b
        xn = ctx.dram([D, T], BF16, "xn"); G = ctx.dram([3 * D, T], F32, "G"); mg = ctx.dram([D, T], BF16, "mg")
        h1 = ctx.dram([D, T], F32, "h1"); qd = ctx.dram([D, T], BF16, "qd"); memn = ctx.dram([D, M], BF16, "memn")
        kTd = ctx.dram([D, M], BF16, "kTd"); vtok = ctx.dram([M, D], BF16, "vtok"); od = ctx.dram([D, T], BF16, "od")
        h2 = ctx.dram([D, T], F32, "h2"); hid = ctx.dram([DFF, T], BF16, "hid")
        hp = [ctx.dram([D, T], F32, "hp%d" % i) for i in range(3)]
        h3 = out if not last else ctx.dram([D, T], F32, "h3")

        norm_fm(ctx, hT, gain(0), xn, T, C)

        def epi_gate(ctx, pl, o, nb, nn, t0, s, SW):
            ctx.op("act", lambda: nc.scalar.activation(out=o.ap[:nn, s * SW:(s + 1) * SW], in_=pl[0].ap[:nn, :SW], func=AF.Sigmoid, bias=bgt.ap[:nn, nb:nb + 1]),
                   outs=[o], ins=[pl[0], bgt], part=(s > 0))
        gemm_fm(ctx, [(Wg, xn, 0, D)], G, 3 * D, T, epi_gate)

        with contextlib.ExitStack() as es2:
            gts = Rot([ctx.sb([128, 512], F32, "gt", es2) for _ in range(6)])
            tms = Rot([ctx.sb([128, 512], F32, "tm", es2) for _ in range(6)])
            def epi_merge(ctx, pl, o, nb, nn, t0, s, SW):
                ts_ = []
                for n in range(3):
                    g = gts.next(); t = tms.next()
                    ctx.dma(("sp", "pool")[n % 2], g.ap[:, :SW], G.ap[n * D + nb * 128:n * D + (nb + 1) * 128, t0:t0 + SW], outs=[g], ins=[G])
                    ctx.op("dve", lambda: nc.vector.tensor_tensor(out=t.ap[:, :SW], in0=pl[n].ap[:, :SW], in1=g.ap[:, :SW], op=ALU.mult), outs=[t], ins=[pl[n], g])
                    ts_.append(t)
                ctx.op("pool", lambda: nc.gpsimd.tensor_tensor(out=ts_[0].ap[:, :SW], in0=ts_[0].ap[:, :SW], in1=ts_[1].ap[:, :SW], op=ALU.add), outs=[ts_[0]], ins=[ts_[0], ts_[1]])
                ctx.op("pool", lambda: nc.gpsimd.tensor_tensor(out=o.ap[:, s * SW:(s + 1) * SW], in0=ts_[0].ap[:, :SW], in1=ts_[2].ap[:, :SW], op=ALU.add), outs=[o], ins=[ts_[0], ts_[2]], part=(s > 0))
            gemm_fm(ctx, [(Wbr[n], yT, n * 1024, 1024) for n in range(3)], mg, D, T, epi_merge)

        def make_epi_add(A, es_):
            ads = Rot([ctx.sb([128, 512], F32, "ad", es_) for _ in range(3)])
            def epi(ctx, pl, o, nb, nn, t0, s, SW):
                a = ads.next()
                ctx.dma(("sp", "pool")[nb % 2], a.ap[:, :SW], A.ap[nb * 128:(nb + 1) * 128, t0:t0 + SW], outs=[a], ins=[A])
                ctx.op("dve", lambda: nc.vector.tensor_tensor(out=o.ap[:, s * SW:(s + 1) * SW], in0=pl[0].ap[:, :SW], in1=a.ap[:, :SW], op=ALU.add),
                       outs=[o], ins=[pl[0], a], part=(s > 0))
            return epi
        def epi_copy(ctx, pl, o, nb, nn, t0, s, SW):
            ctx.op("act", lambda: nc.scalar.copy(out=o.ap[:nn, s * SW:(s + 1) * SW], in_=pl[0].ap[:nn, :SW]), outs=[o], ins=[pl[0]], part=(s > 0))

        with contextlib.ExitStack() as es2:
            gemm_fm(ctx, [(Wout, mg, 0, D)], h1, D, T, make_epi_add(hT, es2))

        norm_fm(ctx, h1, gain(1), xn, T, C)
        gemm_fm(ctx, [(Wq, xn, 0, D)], qd, D, T, epi_copy)
        norm_fm(ctx, memT, gain(2), memn, M, C)
        gemm_fm(ctx, [(Wkv[:, 0:D], memn, 0, D)], kTd, D, M, epi_copy)
        gemm_tm(ctx, Wkv[:, D:2 * D], memn, vtok, D, D, M)
        attention(ctx, qd, kTd, vtok, od, T, C)
        with contextlib.ExitStack() as es2:
            gemm_fm(ctx, [(Wo, od, 0, D)], h2, D, T, make_epi_add(h1, es2))

        norm_fm(ctx, h2, gain(3), xn, T, C)
        with contextlib.ExitStack() as es2:
            rts = Rot([ctx.sb([128, 512], F32, "rt", es2) for _ in range(3)])
            def epi_relu2(ctx, pl, o, nb, nn, t0, s, SW):
                r = rts.next()
                ctx.op("act", lambda: nc.scalar.activation(out=r.ap[:, :SW], in_=pl[0].ap[:, :SW], func=AF.Relu), outs=[r], ins=[pl[0]])
                ctx.op("pool", lambda: nc.gpsimd.tensor_tensor(out=o.ap[:, s * SW:(s + 1) * SW], in0=r.ap[:, :SW], in1=r.ap[:, :SW], op=ALU.mult), outs=[o], ins=[r], part=(s > 0))
            gemm_fm(ctx, [(W1, xn, 0, D)], hid, DFF, T, epi_relu2)
        NG = DFF // D
        dst = [hp[i % 3] for i in range(NG - 1)] + [h3]
        for g in range(NG):
            with contextlib.ExitStack() as es2:
                gemm_fm(ctx, [(W2[g * D:(g + 1) * D, :], hid, g * D, D)], dst[g], D, T, make_epi_add(h2 if g == 0 else dst[g - 1], es2))
        if last:
            gf = gain(4)
            norm_fm(ctx, h3, gf, out, T, C)
        ctx.finish([out])
    return nc


SCAN_STOP = 0
DK = 128
DV = 256
CM = 12582912.0


def col(buf, ap):
    b = Buf(buf.name + "_v", ap); b.w = buf.w; b.r = buf.r; b.wold = buf.wold
    return b


def scan(ctx, C, P, L, kind, bwd, cols, OF, yout, hn_bc, gl=None, yT=None):
    nc = ctx.nc
    ctx.barrier()
    NCH = L // 128
    ml = kind == "ml"
    NV = DV + 8 if ml else DV
    sc = 1.0 if ml else 1.0 / 16.0
    TRI = C["LT"] if bwd else C["UT"]
    STR = C["SUp"] if bwd else C["SLo"]
    ident = C["ident"]; zeros = C["zeros"]; one_c = C["one_col"]
    q0, v0, s0, ns = cols
    with contextlib.ExitStack() as es:
        R = lambda shape, dt, n, k=3: Rot([ctx.sb(shape, dt, n, es) for _ in range(k)])
        qk_r = R([128, 256], F32, "qk"); vg_r = R([128, 512], F32, "vg"); sm_r = R([128, ns], F32, "sm")
        lap_r = R([128, 128], F32, "lap"); ksc_r = R([128, 128], F32, "ksc")
        E1_r = R([128, 128], F32, "E1"); E2_r = R([128, 128], F32, "E2"); E3_r = R([128, 128], F32, "E3")
        qe_r = R([128, 128], BF16, "qe"); ke_r = R([128, 128], BF16, "ke"); kg_r = R([128, 128], BF16, "kg"); at_r = R([128, 128], BF16, "at")
        vx_r = [ctx.sb([128, NV], BF16, "vx", es) for _ in range(3)]
        for vx in vx_r:
            ctx.op("pool", lambda: nc.gpsimd.memset(vx.ap[:], 1.0), outs=[vx], ins=[])
        vx_r = Rot(vx_r)
        c1_r = R([128, 8], F32, "c1"); osb_r = R([128, 256], F32, "osb"); of_r = R([128, 256], F32, "of")
        t1_r = R([128, 256], F32, "t1"); t2_r = R([128, 256], F32, "t2"); yb_r = R([128, 256], BF16, "yb")
        lrT_r = R([16, 128], F32, "lrT", 2); zb_r = R([128, 128], F32, "zb", 2)
        ytb_r = R([128, 2, 128], BF16, "ytb", 2)
        S = ctx.sb([128, NV], F32, "S", es); Sb = ctx.sb([128, NV], BF16, "Sb", es)
        ctx.op("pool", lambda: nc.gpsimd.memset(S.ap[:], 0.0), outs=[S], ins=[])
        ctx.op("pool", lambda: nc.gpsimd.memset(Sb.ap[:], 0.0), outs=[Sb], ins=[])
        banks = []
        for i in range(2):
            A = ctx.ps([128, 512], F32, "bkA", es); B = ctx.ps([128, 512], F32, "bkB", es)
            Co = ctx.ps([128, 512], F32, "bkC", es); Du = ctx.ps([128, 512], F32, "bkD", es)
            banks.append(dict(bT=col(A, A.ap[:, 0:128]), qT=col(A, A.ap[:, 128:256]), kT=col(A, A.ap[:, 256:384]), gx=col(A, A.ap[:, 384:512]),
                              AT=col(B, B.ap[:, 0:128]), lrT=col(B, B.ap[:16, 128:256]), z=col(B, B.ap[:, 256:384]),
                              o=col(Co, Co.ap[:, :NV]), u=col(Du, Du.ap[:, :NV]),
                              tp0=col(B, B.ap[:, 384:512]), tp1=col(Du, Du.ap[:, 384:512])))
        order = range(NCH - 1, -1, -1) if bwd else range(NCH)
        for it, c in enumerate(order):
            bk = banks[it % 2]
            t0 = c * 128
            qk = qk_r.next(); vg = vg_r.next(); sm = sm_r.next()
            ctx.dma("sp", qk.ap[:], P.ap[t0:t0 + 128, q0:q0 + 256], outs=[qk], ins=[P])
            ctx.dma("pool", vg.ap[:], P.ap[t0:t0 + 128, v0:v0 + 512], outs=[vg], ins=[P])
            ctx.dma("sp", sm.ap[:], P.ap[t0:t0 + 128, s0:s0 + ns], outs=[sm], ins=[P])
            if SCAN_STOP == 1:
                continue
            lap = lap_r.next(); c1 = c1_r.next()
            if ml:
                ic = 2 if bwd else 0
                ctx.op("act", lambda: nc.scalar.activation(out=c1.ap[:, 0:1], in_=sm.ap[:, ic:ic + 1], func=AF.Exp), outs=[c1], ins=[sm])
                ctx.op("act", lambda: nc.scalar.activation(out=c1.ap[:, 1:2], in_=sm.ap[:, ic + 1:ic + 2], func=AF.Exp, scale=-1.0), outs=[c1], ins=[sm], part=True)
                ctx.op("act", lambda: nc.scalar.activation(out=c1.ap[:, 2:3], in_=c1.ap[:, 1:2], func=AF.Ln, bias=one_c.ap[:, 0:1]), outs=[c1], ins=[c1, one_c], part=True)
                ctx.op("act", lambda: nc.scalar.activation(out=lap.ap[:], in_=zeros.ap[:, :128], func=AF.Identity, bias=c1.ap[:, 2:3]), outs=[lap], ins=[zeros, c1])
                ksc = ksc_r.next()
                ctx.op("act", lambda: nc.scalar.activation(out=ksc.ap[:], in_=qk.ap[:, 128:256], func=AF.Copy, scale=c1.ap[:, 0:1]), outs=[ksc], ins=[qk, c1])
                k_ap = ksc.ap[:]; kb = ksc
            else:
                dU, dB_bc = gl
                d0 = 16 if bwd else 0
                ctx.op("pe", lambda: nc.tensor.matmul(bk["lrT"].ap, lhsT=sm.ap[:, d0:d0 + 16], rhs=ident.ap[:], start=True, stop=True), outs=[bk["lrT"]], ins=[sm, ident])
                lrT = lrT_r.next()
                ctx.op("dve", lambda: nc.vector.tensor_copy(out=lrT.ap[:], in_=bk["lrT"].ap), outs=[lrT], ins=[bk["lrT"]])
                ctx.op("pe", lambda: nc.tensor.matmul(bk["z"].ap, lhsT=lrT.ap[:], rhs=dU.ap[:, d0 * 8:d0 * 8 + 128], start=True, stop=True), outs=[bk["z"]], ins=[lrT, dU])
                zb = zb_r.next()
                ctx.op("dve", lambda: nc.vector.tensor_tensor(out=zb.ap[:], in0=bk["z"].ap, in1=dB_bc.ap[:, d0 * 8:d0 * 8 + 128], op=ALU.add), outs=[zb], ins=[bk["z"], dB_bc])
                ctx.op("act", lambda: nc.scalar.activation(out=zb.ap[:], in_=zb.ap[:], func=AF.Exp, scale=-1.0), outs=[zb], ins=[zb])
                ctx.op("act", lambda: nc.scalar.activation(out=lap.ap[:], in_=zb.ap[:], func=AF.Ln, bias=one_c.ap[:, 0:1]), outs=[lap], ins=[zb, one_c])
                k_ap = qk.ap[:, 128:256]; kb = qk
            if SCAN_STOP == 2:
                continue
            ctx.op("pe", lambda: nc.tensor.matmul(bk["bT"].ap, lhsT=lap.ap[:], rhs=TRI.ap[:], start=True, stop=True), outs=[bk["bT"]], ins=[lap, TRI])
            ctx.op("pe", lambda: nc.tensor.matmul(bk["qT"].ap, lhsT=qk.ap[:, 0:128], rhs=ident.ap[:], start=True, stop=True), outs=[bk["qT"]], ins=[qk, ident], part=True)
            ctx.op("pe", lambda: nc.tensor.matmul(bk["kT"].ap, lhsT=k_ap, rhs=ident.ap[:], start=True, stop=True), outs=[bk["kT"]], ins=[kb, ident], part=True)
            ctx.op("pe", lambda: nc.tensor.matmul(bk["gx"].ap, lhsT=STR.ap[:], rhs=lap.ap[:], start=True, stop=True), outs=[bk["gx"]], ins=[STR, lap], part=True)
            if SCAN_STOP == 3:
                continue
            E1 = E1_r.next(); E2 = E2_r.next(); E3 = E3_r.next()
            ctx.op("dve", lambda: nc.vector.tensor_copy(out=E1.ap[:], in_=bk["bT"].ap), outs=[E1], ins=[bk["bT"]])
            ctx.op("dve", lambda: nc.vector.tensor_copy(out=E3.ap[:], in_=bk["gx"].ap), outs=[E3], ins=[bk["gx"]])
            ctx.op("act", lambda: nc.scalar.activation(out=E2.ap[:], in_=E1.ap[:], func=AF.Exp, scale=sc), outs=[E2], ins=[E1])
            ctx.op("act", lambda: nc.scalar.activation(out=E1.ap[:], in_=E1.ap[:], func=AF.Exp, scale=-sc), outs=[E1], ins=[E1])
            ctx.op("act", lambda: nc.scalar.activation(out=E3.ap[:], in_=E3.ap[:], func=AF.Exp, scale=-sc), outs=[E3], ins=[E3])
            if SCAN_STOP == 4:
                continue
            qe = qe_r.next(); ke = ke_r.next(); kg = kg_r.next(); at = at_r.next(); vx = vx_r.next()
            ctx.op("dve", lambda: nc.vector.scalar_tensor_tensor(out=qe.ap[:], in0=bk["qT"].ap, scalar=DK ** -0.5, in1=E1.ap[:], op0=ALU.mult, op1=ALU.mult), outs=[qe], ins=[bk["qT"], E1])
            ctx.op("dve", lambda: nc.vector.tensor_tensor(out=ke.ap[:], in0=bk["kT"].ap, in1=E2.ap[:], op=ALU.mult), outs=[ke], ins=[bk["kT"], E2])
            ctx.op("pool", lambda: nc.gpsimd.tensor_tensor(out=kg.ap[:], in0=k_ap, in1=E3.ap[:], op=ALU.mult), outs=[kg], ins=[kb, E3])
            ctx.op("pool", lambda: nc.gpsimd.tensor_copy(out=vx.ap[:, :DV], in_=vg.ap[:, :DV]), outs=[vx], ins=[vg], part=True)
            if SCAN_STOP == 5:
                continue
            ctx.op("pe", lambda: nc.tensor.matmul(bk["AT"].ap, lhsT=ke.ap[:], rhs=qe.ap[:], start=True, stop=True), outs=[bk["AT"]], ins=[ke, qe])
            ctx.op("dve", lambda: nc.vector.tensor_tensor(out=at.ap[:], in0=bk["AT"].ap, in1=TRI.ap[:], op=ALU.mult), outs=[at], ins=[bk["AT"], TRI])
            if SCAN_STOP == 6:
                continue
            ctx.op("pe", lambda: nc.tensor.matmul(bk["o"].ap, lhsT=at.ap[:], rhs=vx.ap[:], start=True, stop=False), outs=[bk["o"]], ins=[at, vx])
            ctx.op("pe", lambda: nc.tensor.matmul(bk["o"].ap, lhsT=qe.ap[:], rhs=Sb.ap[:], start=False, stop=True), outs=[bk["o"]], ins=[qe, Sb], acc=True)
            ctx.op("pe", lambda: nc.tensor.matmul(bk["u"].ap, lhsT=kg.ap[:], rhs=vx.ap[:], start=True, stop=True), outs=[bk["u"]], ins=[kg, vx])
            if SCAN_STOP == 7:
                continue
            egc = E1.ap[:, 0:1] if bwd else E1.ap[:, 127:128]
            ctx.op("dve", lambda: nc.vector.tensor_scalar(out=S.ap[:], in0=S.ap[:], scalar1=egc, scalar2=None, op0=ALU.mult), outs=[S], ins=[S, E1])
            ctx.op("dve", lambda: nc.vector.tensor_tensor(out=S.ap[:], in0=bk["u"].ap, in1=S.ap[:], op=ALU.add), outs=[S], ins=[S, bk["u"]])
            ctx.op("act", lambda: nc.scalar.copy(out=Sb.ap[:], in_=S.ap[:]), outs=[Sb], ins=[S])
            if SCAN_STOP == 8:
                continue
            osb = osb_r.next()
            if ml:
                ctx.op("dve", lambda: nc.vector.tensor_scalar(out=c1.ap[:, 3:4], in0=bk["o"].ap[:, DV:DV + 1], scalar1=-1.0, scalar2=None, op0=ALU.mult), outs=[c1], ins=[bk["o"]], part=True)
                ctx.op("dve", lambda: nc.vector.tensor_tensor(out=c1.ap[:, 3:4], in0=bk["o"].ap[:, DV:DV + 1], in1=c1.ap[:, 3:4], op=ALU.max), outs=[c1], ins=[bk["o"], c1], part=True)
                ctx.op("dve", lambda: nc.vector.tensor_scalar_max(out=c1.ap[:, 3:4], in0=c1.ap[:, 3:4], scalar1=1.0), outs=[c1], ins=[c1], part=True)
                ctx.op("dve", lambda: nc.vector.reciprocal(out=c1.ap[:, 4:5], in_=c1.ap[:, 3:4]), outs=[c1], ins=[c1], part=True)
                ctx.op("act", lambda: nc.scalar.activation(out=osb.ap[:], in_=bk["o"].ap[:, :DV], func=AF.Copy, scale=c1.ap[:, 4:5]), outs=[osb], ins=[bk["o"], c1])
            else:
                ctx.op("act", lambda: nc.scalar.copy(out=osb.ap[:], in_=bk["o"].ap[:, :DV]), outs=[osb], ins=[bk["o"]])
            if not bwd:
                ctx.dma("pool", OF.ap[t0:t0 + 128, :], osb.ap[:], outs=[OF], ins=[osb], part=True)
                continue
            of = of_r.next(); t1 = t1_r.next(); t2 = t2_r.next(); yb = yb_r.next()
            ctx.dma("sp", of.ap[:], OF.ap[t0:t0 + 128, :], outs=[of], ins=[OF])
            ctx.op("dve", lambda: nc.vector.tensor_tensor(out=osb.ap[:], in0=osb.ap[:], in1=of.ap[:], op=ALU.add), outs=[osb], ins=[osb, of])
            ctx.op("act", lambda: nc.scalar.activation(out=t1.ap[:], in_=osb.ap[:], func=AF.Square, accum_out=c1.ap[:, 5:6]), outs=[t1, c1], ins=[osb], part=True)
            ctx.op("dve", lambda: nc.vector.tensor_scalar(out=c1.ap[:, 6:7], in0=c1.ap[:, 5:6], scalar1=1.0 / DV, scalar2=EPS, op0=ALU.mult, op1=ALU.add), outs=[c1], ins=[c1], part=True)
            ctx.op("act", lambda: nc.scalar.sqrt(out=c1.ap[:, 6:7], in_=c1.ap[:, 6:7]), outs=[c1], ins=[c1], part=True)
            ctx.op("dve", lambda: nc.vector.reciprocal(out=c1.ap[:, 7:8], in_=c1.ap[:, 6:7]), outs=[c1], ins=[c1], part=True)
            ctx.op("act", lambda: nc.scalar.activation(out=t2.ap[:], in_=vg.ap[:, DV:2 * DV], func=AF.Exp, scale=-1.0), outs=[t2], ins=[vg])
            ctx.op("pool", lambda: nc.gpsimd.tensor_scalar_add(out=t2.ap[:], in0=t2.ap[:], scalar1=1.0), outs=[t2], ins=[t2])
            ctx.op("dve", lambda: nc.vector.reciprocal(out=t2.ap[:], in_=t2.ap[:]), outs=[t2], ins=[t2])
            if not ml:
                ctx.op("pool", lambda: nc.gpsimd.tensor_tensor(out=t2.ap[:], in0=t2.ap[:], in1=vg.ap[:, DV:2 * DV], op=ALU.mult), outs=[t2], ins=[t2, vg])
            ctx.op("dve", lambda: nc.vector.scalar_tensor_tensor(out=t1.ap[:], in0=osb.ap[:], scalar=c1.ap[:, 7:8], in1=hn_bc, op0=ALU.mult, op1=ALU.mult), outs=[t1], ins=[osb, c1, C["hn"]])
            ctx.op("pool", lambda: nc.gpsimd.tensor_tensor(out=yb.ap[:], in0=t1.ap[:], in1=t2.ap[:], op=ALU.mult), outs=[yb], ins=[t1, t2])
            if yT is None:
                ctx.dma("pool", yout.ap[t0:t0 + 128, :], yb.ap[:], outs=[yout], ins=[yb], part=True)
            else:
                YT, row0 = yT
                ytb = ytb_r.next()
                for hh in range(2):
                    pt = bk["tp%d" % hh]
                    ctx.op("pe", lambda: nc.tensor.matmul(pt.ap, lhsT=yb.ap[:, hh * 128:(hh + 1) * 128], rhs=C["ident_bf"].ap[:], start=True, stop=True), outs=[pt], ins=[yb, C["ident_bf"]], part=True)
                    ctx.op("dve", lambda: nc.vector.tensor_copy(out=ytb.ap[:, hh, :], in_=pt.ap), outs=[ytb], ins=[pt], part=(hh > 0))
                ctx.dma("pool", YT.ap[row0:row0 + 256, t0:t0 + 128].rearrange("(h p) t -> p h t", p=128), ytb.ap[:], outs=[YT], ins=[ytb], part=True)


def sin_layer(ctx, nc, ps, freq, fb, out, tmp, np_, w):
    x, k = tmp
    ctx.op("dve", lambda: nc.vector.tensor_scalar(out=x.ap[:np_, :w], in0=ps.ap[:np_, :w], scalar1=freq, scalar2=fb, op0=ALU.mult, op1=ALU.add), outs=[x], ins=[ps])
    ctx.op("dve", lambda: nc.vector.tensor_scalar(out=k.ap[:np_, :w], in0=x.ap[:np_, :w], scalar1=1.0 / (2 * np.pi), scalar2=CM, op0=ALU.mult, op1=ALU.add), outs=[k], ins=[x])
    ctx.op("dve", lambda: nc.vector.tensor_scalar_add(out=k.ap[:np_, :w], in0=k.ap[:np_, :w], scalar1=-CM), outs=[k], ins=[k])
    ctx.op("dve", lambda: nc.vector.scalar_tensor_tensor(out=x.ap[:np_, :w], in0=k.ap[:np_, :w], scalar=-2 * np.pi, in1=x.ap[:np_, :w], op0=ALU.mult, op1=ALU.add), outs=[x], ins=[k, x])
    ctx.op("act", lambda: nc.scalar.activation(out=out.ap[:np_, :w], in_=x.ap[:np_, :w], func=AF.Sin), outs=[out], ins=[x])


def hyena(ctx, C, HY, U, FILT, KS, Z1, yhy, L, hin, upto=9):
    nc = ctx.nc
    N1 = 2 * L // 128
    NH1 = N1 // 2
    NT = L // 512
    ctx.barrier()
    with contextlib.ExitStack() as es:
        xs = Rot([ctx.sb([128, L + 2], F32, "hx", es) for _ in range(2)])
        acc = ctx.sb([128, L], F32, "hacc", es)
        cw = hin["cw"]; cb = hin["cb"]
        for blk in range(6):
            x = xs.next()
            ctx.op("pool", lambda: nc.gpsimd.memset(x.ap[:, 0:1], 0.0), outs=[x], ins=[])
            ctx.op("pool", lambda: nc.gpsimd.memset(x.ap[:, L + 1:L + 2], 0.0), outs=[x], ins=[], part=True)
            ctx.dma(("sp", "pool")[blk % 2], x.ap[:, 1:L + 1], HY.ap[blk * 128:(blk + 1) * 128, :], outs=[x], ins=[HY], part=True)
            ctx.op("dve", lambda: nc.vector.tensor_scalar(out=acc.ap[:], in0=x.ap[:, 1:L + 1], scalar1=cw.ap[:, blk, 1:2], scalar2=cb.ap[:, blk:blk + 1], op0=ALU.mult, op1=ALU.add), outs=[acc], ins=[x, cw, cb])
            ctx.op("dve", lambda: nc.vector.scalar_tensor_tensor(out=acc.ap[:], in0=x.ap[:, 0:L], scalar=cw.ap[:, blk, 0:1], in1=acc.ap[:], op0=ALU.mult, op1=ALU.add), outs=[acc], ins=[x, cw, acc])
            ctx.op("dve", lambda: nc.vector.scalar_tensor_tensor(out=acc.ap[:], in0=x.ap[:, 2:L + 2], scalar=cw.ap[:, blk, 2:3], in1=acc.ap[:], op0=ALU.mult, op1=ALU.add), outs=[acc], ins=[x, cw, acc])
            ctx.dma(("sp", "pool")[(blk + 1) % 2], U.ap[blk * 128:(blk + 1) * 128, :], acc.ap[:], outs=[U], ins=[acc], part=True)
    if upto < 7:
        return
    ctx.barrier()
    with contextlib.ExitStack() as es:
        zt = Rot([ctx.sb([33, 512], F32, "fz", es) for _ in range(2)])
        h1 = ctx.sb([64, 512], F32, "fh1", es); h2 = ctx.sb([64, 512], F32, "fh2", es)
        tx = ctx.sb([64, 512], F32, "ftx", es); tk = ctx.sb([64, 512], F32, "ftk", es)
        wt = Rot([ctx.sb([128, 512], F32, "fwin", es) for _ in range(4)])
        ff = Rot([ctx.sb([128, 512], F32, "ff", es) for _ in range(3)])
        fo = Rot([ctx.sb([128, 512], BF16, "fo", es) for _ in range(3)])
        ps1 = ctx.ps([128, 512], F32, "fps1", es); ps2 = ctx.ps([128, 512], F32, "fps2", es)
        ps3 = Rot([ctx.ps([128, 512], F32, "fps3", es) for _ in range(3)])
        fw1, fw2, fw3, fq, fb1, fb2, skipc = hin["fw1"], hin["fw2"], hin["fw3"], hin["ffreq"], hin["ffb1"], hin["ffb2"], hin["skipc"]
        for tt in range(NT):
            z = zt.next()
            ctx.dma("sp", z.ap[:], hin["zT"].ap[:, tt * 512:(tt + 1) * 512], outs=[z], ins=[hin["zT"]])
            ctx.op("pe", lambda: nc.tensor.matmul(ps1.ap[:64, :], lhsT=fw1.ap[:], rhs=z.ap[:], start=True, stop=True), outs=[ps1], ins=[fw1, z])
            sin_layer(ctx, nc, ps1, fq.ap[:, 0:1], fb1.ap[:, 0:1], h1, (tx, tk), 64, 512)
            ctx.op("pe", lambda: nc.tensor.matmul(ps2.ap[:64, :], lhsT=fw2.ap[:], rhs=h1.ap[:], start=True, stop=True), outs=[ps2], ins=[fw2, h1])
            sin_layer(ctx, nc, ps2, fq.ap[:, 0:1], fb2.ap[:, 0:1], h2, (tx, tk), 64, 512)
            wins = []
            for ch in range(2):
                w = wt.next()
                ctx.dma(("sp", "pool")[ch], w.ap[:], hin["win"].ap[ch * 128:(ch + 1) * 128, tt * 512:(tt + 1) * 512], outs=[w], ins=[hin["win"]])
                wins.append(w)
            for cbk in range(8):
                o_, dr, ch = cbk // 4, (cbk // 2) % 2, cbk % 2
                ps = ps3.next(); f = ff.next(); fb = fo.next()
                ctx.op("pe", lambda: nc.tensor.matmul(ps.ap[:], lhsT=fw3.ap[:, cbk * 128:(cbk + 1) * 128], rhs=h2.ap[:], start=True, stop=True), outs=[ps], ins=[fw3, h2])
                ctx.op("dve", lambda: nc.vector.tensor_tensor(out=f.ap[:], in0=ps.ap[:], in1=wins[ch].ap[:], op=ALU.mult), outs=[f], ins=[ps, wins[ch]])
                if tt == 0:
                    if dr == 0:
                        ctx.op("dve", lambda: nc.vector.tensor_tensor(out=f.ap[:, 0:1], in0=f.ap[:, 0:1], in1=skipc.ap[:, o_ * 2 + ch:o_ * 2 + ch + 1], op=ALU.add), outs=[f], ins=[f, skipc])
                    else:
                        ctx.op("dve", lambda: nc.vector.memset(f.ap[:, 0:1], 0.0), outs=[f], ins=[f])
                ctx.op("pool", lambda: nc.gpsimd.tensor_copy(out=fb.ap[:], in_=f.ap[:]), outs=[fb], ins=[f])
                ctx.dma(("sp", "pool")[cbk % 2], FILT.ap[cbk * 128:(cbk + 1) * 128, tt * 512:(tt + 1) * 512], fb.ap[:], outs=[FILT], ins=[fb], part=True)
    if upto < 8:
        return
    ctx.barrier()
    with contextlib.ExitStack() as es:
        FA, TWr, TWi, FBr, FBi, FBin = hin["FA"], hin["TWr"], hin["TWi"], hin["FBr"], hin["FBi"], hin["FBin"]
        GC1, GC2, TWTr, TWTi, GAr, GAin = hin["GC1"], hin["GC2"], hin["TWTr"], hin["TWTi"], hin["GAr"], hin["GAin"]
        psA = [ctx.ps([128, 512], F32, "psA", es) for _ in range(2)]
        psXr = ctx.ps([128, 512], F32, "psXr", es); psXi = ctx.ps([128, 512], F32, "psXi", es)
        psC = [ctx.ps([128, 512], F32, "psC", es) for _ in range(2)]
        psD = ctx.ps([128, 512], F32, "psD", es)
        W4 = 4 * N1
        ap_r = Rot([ctx.sb([128, 4, N1], BF16, "apr", es) for _ in range(2)]); ap_i = Rot([ctx.sb([128, 4, N1], BF16, "api", es) for _ in range(2)])
        tmp = Rot([ctx.sb([128, 2, 128], F32, "ftmp", es) for _ in range(8)])
        tmp4 = Rot([ctx.sb([128, 4, N1], F32, "ftmp4", es) for _ in range(6)])
        y_r = Rot([ctx.sb([128, 4, N1], BF16, "yr", es) for _ in range(2)]); y_i = Rot([ctx.sb([128, 4, N1], BF16, "yi", es) for _ in range(2)])
        bp_r = Rot([ctx.sb([N1, 4, 128], BF16, "bpr", es) for _ in range(2)]); bp_i = Rot([ctx.sb([N1, 4, 128], BF16, "bpi", es) for _ in range(2)])
        srcf = Rot([ctx.sb([NH1, 16, 128], F32, "srcf", es) for _ in range(2)])
        srcb = Rot([ctx.sb([NH1, 32, 128], BF16, "srcb", es) for _ in range(2)])
        gatef = Rot([ctx.sb([NH1, 16, 128], F32, "gatef", es) for _ in range(2)])
        Kt = Rot([ctx.sb([128, 2, 16, N1], F32, "Kt", es) for _ in range(2)])
        outf = Rot([ctx.sb([NH1, 16, 128], F32, "outf", es) for _ in range(2)])
        outb = Rot([ctx.sb([NH1, 16, 128], BF16, "outb", es) for _ in range(2)])
        TWr_b = TWr.ap[:].unsqueeze(1).to_broadcast([128, 2, N1]); TWi_b = TWi.ap[:].unsqueeze(1).to_broadcast([128, 2, N1])
        TTr_b = TWTr.ap[:].unsqueeze(1).to_broadcast([N1, 2, 128]); TTi_b = TWTi.ap[:].unsqueeze(1).to_broadcast([N1, 2, 128])

        def cmul_bank(bank, npart, w, Tr_b, Ti_b, Tb, obr, out_r, obi, out_i, conj, part):
            v = bank.ap[:npart, :4 * w].rearrange("p (s c k) -> p s c k", s=2, c=2)
            Ar = v[:, :, 0, :]; Ai = v[:, :, 1, :]
            t = [tmp.next() for _ in range(4)]
            tv = [x.ap[:npart, :, :w] for x in t]
            ctx.op("dve", lambda: nc.vector.tensor_tensor(out=tv[0], in0=Ar, in1=Tr_b, op=ALU.mult), outs=[t[0]], ins=[bank] + Tb)
            ctx.op("dve", lambda: nc.vector.tensor_tensor(out=tv[1], in0=Ai, in1=Ti_b, op=ALU.mult), outs=[t[1]], ins=[bank] + Tb)
            ctx.op("dve", lambda: nc.vector.tensor_tensor(out=tv[2], in0=Ar, in1=Ti_b, op=ALU.mult), outs=[t[2]], ins=[bank] + Tb)
            ctx.op("dve", lambda: nc.vector.tensor_tensor(out=tv[3], in0=Ai, in1=Tr_b, op=ALU.mult), outs=[t[3]], ins=[bank] + Tb)
            if not conj:
                ctx.op("pool", lambda: nc.gpsimd.tensor_tensor(out=out_r, in0=tv[0], in1=tv[1], op=ALU.subtract), outs=[obr], ins=[t[0], t[1]], part=part)
                ctx.op("pool", lambda: nc.gpsimd.tensor_tensor(out=out_i, in0=tv[2], in1=tv[3], op=ALU.add), outs=[obi], ins=[t[2], t[3]], part=part)
            else:
                ctx.op("pool", lambda: nc.gpsimd.tensor_tensor(out=out_r, in0=tv[0], in1=tv[1], op=ALU.add), outs=[obr], ins=[t[0], t[1]], part=part)
                ctx.op("pool", lambda: nc.gpsimd.tensor_tensor(out=out_i, in0=tv[3], in1=tv[2], op=ALU.subtract), outs=[obi], ins=[t[2], t[3]], part=part)

        def fwd_transform(src, idx):
            ar = ap_r.next(); ai = ap_i.next()
            for s in range(4):
                bank = psA[s // 2]
                ctx.op("pe", lambda: nc.tensor.matmul(bank.ap[:, (s % 2) * 2 * N1:(s % 2 + 1) * 2 * N1], lhsT=src.ap[:, idx[s], :], rhs=FA.ap[:], start=True, stop=True),
                       outs=[bank], ins=[src, FA], part=(s % 2 == 1))
            for b2 in range(2):
                cmul_bank(psA[b2], 128, N1, TWr_b, TWi_b, [TWr, TWi], ar, ar.ap[:, b2 * 2:(b2 + 1) * 2, :], ai, ai.ap[:, b2 * 2:(b2 + 1) * 2, :], False, b2 > 0)
            arf = ar.ap[:].rearrange("p s k -> p (s k)"); aif = ai.ap[:].rearrange("p s k -> p (s k)")
            ctx.op("pe", lambda: nc.tensor.matmul(psXr.ap[:, :W4], lhsT=FBr.ap[:], rhs=arf, start=True, stop=False), outs=[psXr], ins=[FBr, ar])
            ctx.op("pe", lambda: nc.tensor.matmul(psXr.ap[:, :W4], lhsT=FBin.ap[:], rhs=aif, start=False, stop=True), outs=[psXr], ins=[FBin, ai], acc=True)
            ctx.op("pe", lambda: nc.tensor.matmul(psXi.ap[:, :W4], lhsT=FBr.ap[:], rhs=aif, start=True, stop=False), outs=[psXi], ins=[FBr, ai])
            ctx.op("pe", lambda: nc.tensor.matmul(psXi.ap[:, :W4], lhsT=FBi.ap[:], rhs=arf, start=False, stop=True), outs=[psXi], ins=[FBi, ar], acc=True)

        for o_ in range(2):
            for c0 in range(0, 256, 16):
                sb_ = srcb.next()
                rf = (o_ * 2 + 0) * 256 + c0; rb = (o_ * 2 + 1) * 256 + c0
                ctx.dma("sp", sb_.ap[:, 0:16, :], FILT.ap[rf:rf + 16, :].rearrange("c (a b) -> a c b", b=128), outs=[sb_], ins=[FILT])
                ctx.dma("pool", sb_.ap[:, 16:32, :], FILT.ap[rb:rb + 16, :].rearrange("c (a b) -> a c b", b=128), outs=[sb_], ins=[FILT], part=True)
                kt = Kt.next()
                first = True
                for g in range(0, 16, 2):
                    fwd_transform(sb_, [g, g + 1, 16 + g, 17 + g])
                    xr = psXr.ap[:, :W4].rearrange("p (s k) -> p s k", s=4); xi = psXi.ap[:, :W4].rearrange("p (s k) -> p s k", s=4)
                    t = tmp4.next(); t2 = tmp4.next()
                    ctx.op("dve", lambda: nc.vector.tensor_copy(out=t.ap[:, 0:2, :], in_=xr[:, 2:4, :]), outs=[t], ins=[psXr])
                    ctx.op("dve", lambda: nc.vector.tensor_copy(out=t2.ap[:, 0:2, :], in_=xi[:, 2:4, :]), outs=[t2], ins=[psXi])
                    ctx.op("dve", lambda: nc.vector.tensor_tensor(out=kt.ap[:, 0, g:g + 2, :], in0=xr[:, 0:2, :], in1=t.ap[:, 0:2, :], op=ALU.add), outs=[kt], ins=[psXr, t], part=not first)
                    ctx.op("dve", lambda: nc.vector.tensor_tensor(out=kt.ap[:, 1, g:g + 2, :], in0=xi[:, 0:2, :], in1=t2.ap[:, 0:2, :], op=ALU.subtract), outs=[kt], ins=[psXi, t2], part=True)
                    first = False
                for ri in range(2):
                    ctx.dma(("sp", "pool")[ri], KS.ap[o_, ri, :, c0:c0 + 16, :], kt.ap[:, ri, :, :], outs=[KS], ins=[kt], part=True)
            src_d = U if o_ == 0 else Z1
            gate_r0 = 256 if o_ == 0 else 512
            for c0 in range(0, 256, 16):
                sf = srcf.next(); sb_ = srcb.next(); gt = gatef.next(); kt = Kt.next()
                ctx.dma("sp", sf.ap[:], src_d.ap[c0:c0 + 16, :].rearrange("c (a b) -> a c b", b=128), outs=[sf], ins=[src_d])
                ctx.dma("pool", gt.ap[:], U.ap[gate_r0 + c0:gate_r0 + c0 + 16, :].rearrange("c (a b) -> a c b", b=128), outs=[gt], ins=[U])
                for ri in range(2):
                    ctx.dma(("sp", "pool")[ri], kt.ap[:, ri, :, :], KS.ap[o_, ri, :, c0:c0 + 16, :], outs=[kt], ins=[KS], part=(ri > 0))
                ctx.op("act", lambda: nc.scalar.copy(out=sb_.ap[:, 0:16, :], in_=sf.ap[:]), outs=[sb_], ins=[sf])
                ob = outf.next() if o_ == 0 else outb.next()
                for g in range(0, 16, 4):
                    fwd_transform(sb_, [g, g + 1, g + 2, g + 3])
                    xr = psXr.ap[:, :W4].rearrange("p (s k) -> p s k", s=4); xi = psXi.ap[:, :W4].rearrange("p (s k) -> p s k", s=4)
                    kr = kt.ap[:, 0, g:g + 4, :]; ki = kt.ap[:, 1, g:g + 4, :]
                    t = [tmp4.next() for _ in range(4)]
                    ctx.op("dve", lambda: nc.vector.tensor_tensor(out=t[0].ap[:], in0=xr, in1=kr, op=ALU.mult), outs=[t[0]], ins=[psXr, kt])
                    ctx.op("dve", lambda: nc.vector.tensor_tensor(out=t[1].ap[:], in0=xi, in1=ki, op=ALU.mult), outs=[t[1]], ins=[psXi, kt])
                    ctx.op("dve", lambda: nc.vector.tensor_tensor(out=t[2].ap[:], in0=xr, in1=ki, op=ALU.mult), outs=[t[2]], ins=[psXr, kt])
                    ctx.op("dve", lambda: nc.vector.tensor_tensor(out=t[3].ap[:], in0=xi, in1=kr, op=ALU.mult), outs=[t[3]], ins=[psXi, kt])
                    yr = y_r.next(); yi = y_i.next()
                    ctx.op("pool", lambda: nc.gpsimd.tensor_tensor(out=yr.ap[:], in0=t[0].ap[:], in1=t[1].ap[:], op=ALU.subtract), outs=[yr], ins=[t[0], t[1]])
                    ctx.op("pool", lambda: nc.gpsimd.tensor_tensor(out=yi.ap[:], in0=t[2].ap[:], in1=t[3].ap[:], op=ALU.add), outs=[yi], ins=[t[2], t[3]])
                    for s in range(4):
                        bank = psC[s // 2]
                        oc = bank.ap[:N1, (s % 2) * 256:(s % 2 + 1) * 256]
                        ctx.op("pe", lambda: nc.tensor.matmul(oc, lhsT=yr.ap[:, s, :], rhs=GC1.ap[:], start=True, stop=False), outs=[bank], ins=[yr, GC1], part=(s % 2 == 1))
                        ctx.op("pe", lambda: nc.tensor.matmul(oc, lhsT=yi.ap[:, s, :], rhs=GC2.ap[:], start=False, stop=True), outs=[bank], ins=[yi, GC2], acc=True)
                    br = bp_r.next(); bi = bp_i.next()
                    for b2 in range(2):
                        cmul_bank(psC[b2], N1, 128, TTr_b, TTi_b, [TWTr, TWTi], br, br.ap[:, b2 * 2:(b2 + 1) * 2, :], bi, bi.ap[:, b2 * 2:(b2 + 1) * 2, :], True, b2 > 0)
                    ctx.op("pe", lambda: nc.tensor.matmul(psD.ap[:NH1, :], lhsT=GAr.ap[:], rhs=br.ap[:].rearrange("p s k -> p (s k)"), start=True, stop=False), outs=[psD], ins=[GAr, br])
                    ctx.op("pe", lambda: nc.tensor.matmul(psD.ap[:NH1, :], lhsT=GAin.ap[:], rhs=bi.ap[:].rearrange("p s k -> p (s k)"), start=False, stop=True), outs=[psD], ins=[GAin, bi], acc=True)
                    ctx.op("dve", lambda: nc.vector.scalar_tensor_tensor(out=ob.ap[:, g:g + 4, :], in0=psD.ap[:NH1, :].rearrange("p (s k) -> p s k", s=4), scalar=1.0 / (2 * L),
                                                                         in1=gt.ap[:, g:g + 4, :], op0=ALU.mult, op1=ALU.mult), outs=[ob], ins=[psD, gt], part=(g > 0))
                dst = Z1 if o_ == 0 else yhy
                ctx.dma("sp", dst.ap[c0:c0 + 16, :].rearrange("c (a b) -> a c b", b=128), ob.ap[:], outs=[dst], ins=[ob], part=True)


NTOK = 1572
C_MLQ, C_MLV, C_MLG = 0, 256, 768
C_GQ, C_GV, C_GLR = 772, 1028, 1540


def build_A(L, dbg=(), upto=9):
    N1 = 2 * L // 128
    NH1 = N1 // 2
    nc = bass.Bass("TRN2", target_bir_lowering=False)
    def din(name, shape, dt=F32):
        return nc.dram_tensor(name, list(shape), dt, kind="ExternalInput").ap()
    hT = Buf("hT", din("hT", [D, L]))
    g0 = din("g0", [128, 32])
    Wtok = din("Wtok", [D, NTOK]); btok = din("btok", [1, NTOK])
    Why = din("Why", [D, 768]); bhy = din("bhy", [128, 6])
    hn = din("hn", [1, 512]); dU = din("dU", [16, 256]); dB = din("dB", [1, 256])
    cw = din("cw", [128, 6, 3]); cb = din("cb", [128, 6])
    fw1 = din("fw1", [33, 64]); fb1 = din("fb1", [64, 1]); fw2 = din("fw2", [64, 64]); fb2 = din("fb2", [64, 1])
    fw3 = din("fw3", [64, 1024]); ffreq = din("ffreq", [64, 1]); skipc = din("skipc", [128, 4])
    cmats = din("cmats", [128, 5, 128])
    zT = Buf("zT", din("zT", [33, L])); win = Buf("win", din("win", [256, L]))
    FA = din("FA", [NH1, 2 * N1], BF16); TW = din("TW", [128, 2, N1]); FB = din("FB", [128, 3, 128], BF16)
    GC = din("GC", [128, 2, 256], BF16); TWT = din("TWT", [N1, 2, 128]); GA = din("GA", [N1, 2, NH1], BF16)
    def dout(name, shape, dt):
        return Buf(name, nc.dram_tensor(name, list(shape), dt, kind="ExternalOutput").ap())
    y_ml = dout("y_ml", [L, 256], BF16); y_gla = dout("y_gla", [L, 256], BF16); y_hy = dout("y_hy", [256, L], BF16)
    with contextlib.ExitStack() as es:
        ctx = Ctx(nc, es)
        ctx.dbg = dbg
        C = make_consts(ctx, None)
        cm = load_const(ctx, cmats, [128, 5, 128], F32, "cmats")
        for i, n in enumerate(["ident", "UT", "LT", "SLo", "SUp"]):
            C[n] = col(cm, cm.ap[:, i, :])
        C["zeros"] = ctx.sb([128, 128], F32, "zeros")
        ctx.op("pool", lambda: nc.gpsimd.memset(C["zeros"].ap[:], 0.0), outs=[C["zeros"]], ins=[])
        C["one_col"] = ctx.sb([128, 1], F32, "one_col")
        ctx.op("pool", lambda: nc.gpsimd.memset(C["one_col"].ap[:], 1.0), outs=[C["one_col"]], ins=[])
        g0t = load_const(ctx, g0, [128, 32], F32, "g0")
        btok_bc = load_const(ctx, btok.partition_broadcast(128), [128, NTOK], F32, "btok_bc", q="pool")
        bhyt = load_const(ctx, bhy, [128, 6], F32, "bhy")
        C["hn"] = load_const(ctx, hn.partition_broadcast(128), [128, 512], F32, "hn_bc", q="pool")
        dUt = load_const(ctx, dU, [16, 256], F32, "dU"); dB_bc = load_const(ctx, dB.partition_broadcast(128), [128, 256], F32, "dB_bc", q="pool")
        hin = dict(cw=load_const(ctx, cw, [128, 6, 3], F32, "cw"), cb=load_const(ctx, cb, [128, 6], F32, "cb"),
                   fw1=load_const(ctx, fw1, [33, 64], F32, "fw1"), fw2=load_const(ctx, fw2, [64, 64], F32, "fw2"),
                   fw3=load_const(ctx, fw3, [64, 1024], F32, "fw3"), ffreq=load_const(ctx, ffreq, [64, 1], F32, "ffreq"),
                   skipc=load_const(ctx, skipc, [128, 4], F32, "skipc"), zT=zT, win=win,
                   FA=load_const(ctx, FA, [NH1, 2 * N1], BF16, "FA"))
        b1t = load_const(ctx, fb1, [64, 1], F32, "fb1"); b2t = load_const(ctx, fb2, [64, 1], F32, "fb2")
        hin["ffb1"] = ctx.sb([64, 1], F32, "ffb1"); hin["ffb2"] = ctx.sb([64, 1], F32, "ffb2")
        ctx.op("dve", lambda: nc.vector.tensor_tensor(out=hin["ffb1"].ap[:], in0=b1t.ap[:], in1=hin["ffreq"].ap[:], op=ALU.mult), outs=[hin["ffb1"]], ins=[b1t, hin["ffreq"]])
        ctx.op("dve", lambda: nc.vector.tensor_tensor(out=hin["ffb2"].ap[:], in0=b2t.ap[:], in1=hin["ffreq"].ap[:], op=ALU.mult), outs=[hin["ffb2"]], ins=[b2t, hin["ffreq"]])
        twt = load_const(ctx, TW, [128, 2, N1], F32, "TW"); hin["TWr"] = col(twt, twt.ap[:, 0, :]); hin["TWi"] = col(twt, twt.ap[:, 1, :])
        fbt = load_const(ctx, FB, [128, 3, 128], BF16, "FB")
        hin["FBr"] = col(fbt, fbt.ap[:, 0, :]); hin["FBi"] = col(fbt, fbt.ap[:, 1, :]); hin["FBin"] = col(fbt, fbt.ap[:, 2, :])
        gct = load_const(ctx, GC, [128, 2, 256], BF16, "GC"); hin["GC1"] = col(gct, gct.ap[:, 0, :]); hin["GC2"] = col(gct, gct.ap[:, 1, :])
        twtt = load_const(ctx, TWT, [N1, 2, 128], F32, "TWT"); hin["TWTr"] = col(twtt, twtt.ap[:, 0, :]); hin["TWTi"] = col(twtt, twtt.ap[:, 1, :])
        gat = load_const(ctx, GA, [N1, 2, NH1], BF16, "GA"); hin["GAr"] = col(gat, gat.ap[:, 0, :]); hin["GAin"] = col(gat, gat.ap[:, 1, :])

        xn = ctx.dram([D, L], BF16, "xn"); P = ctx.dram([L, NTOK], F32, "P"); HY = ctx.dram([768, L], F32, "HY")
        U = ctx.dram([768, L], F32, "U"); FILT = ctx.dram([1024, L], BF16, "FILT"); KS = ctx.dram([2, 2, 128, 256, N1], F32, "KS")
        Z1 = ctx.dram([256, L], F32, "Z1"); OFm = ctx.dram([L, 256], F32, "OFm"); OFg = ctx.dram([L, 256], F32, "OFg")

        norm_fm(ctx, hT, g0t, xn, L, C)
        if upto >= 2:
            gemm_tm(ctx, Wtok, xn, P, D, NTOK, L, bias_bc=btok_bc)
        def epi_bias(ctx, pl, o, nb, nn, t0, s, SW):
            ctx.op("act", lambda: nc.scalar.activation(out=o.ap[:nn, s * SW:(s + 1) * SW], in_=pl[0].ap[:nn, :SW], func=AF.Identity, bias=bhyt.ap[:nn, nb:nb + 1]),
                   outs=[o], ins=[pl[0], bhyt], part=(s > 0))
        if upto >= 3:
            gemm_fm(ctx, [(Why, xn, 0, D)], HY, 768, L, epi_bias)
        for bwd in (False, True):
            if upto >= 4:
                scan(ctx, C, P, L, "ml", bwd, (C_MLQ, C_MLV, C_MLG, 4), OFm, y_ml, C["hn"].ap[:, 0:256])
        for bwd in (False, True):
            if upto >= 5:
                scan(ctx, C, P, L, "gla", bwd, (C_GQ, C_GV, C_GLR, 32), OFg, y_gla, C["hn"].ap[:, 256:512], gl=(dUt, dB_bc))
        if upto >= 6:
            hyena(ctx, C, HY, U, FILT, KS, Z1, y_hy, L, hin, upto)
        ctx.barrier(["sp"])
    return nc


def consts_A(L, j):
    bf = ml_dtypes.bfloat16
    N1 = 2 * L // 128; NH1 = N1 // 2; N = 2 * L
    i = np.arange(128)
    ident = np.eye(128, dtype=np.float32)
    UT = (i[:, None] <= i[None, :]).astype(np.float32); LT = (i[:, None] >= i[None, :]).astype(np.float32)
    cm = np.stack([ident, UT, LT, LT - ident, UT - ident], axis=1)
    n = np.arange(L, dtype=np.float32)
    t = n / np.float32(L - 1)
    bands = np.linspace(1e-4, 15, 16, dtype=np.float32)
    ang = (np.float32(2.0 * np.pi) * n / np.float32(L))[:, None] * bands[None, :]
    z = np.concatenate([t[:, None], np.cos(ang), -np.sin(ang)], axis=-1).astype(np.float32)
    dmin = np.log(1e-2) / 1.5; dmax = np.log(1e-2) / 0.3
    deltas = np.abs(np.linspace(dmin, dmax, 1024, dtype=np.float32))[j * 256:(j + 1) * 256]
    win = np.exp(-t[None, :] * deltas[:, None]).astype(np.float32)
    n1 = np.arange(NH1)[:, None]; k1 = np.arange(N1)[None, :]
    a = 2 * np.pi * n1 * k1 / N1
    FA = np.concatenate([np.cos(a), -np.sin(a)], axis=1).astype(bf)
    n2 = np.arange(128)[:, None]
    a = 2 * np.pi * n2 * k1 / N
    TW = np.stack([np.cos(a), -np.sin(a)], axis=1).astype(np.float32)
    TWT = np.stack([np.cos(a).T, -np.sin(a).T], axis=1).astype(np.float32)
    a = 2 * np.pi * n2 * np.arange(128)[None, :] / 128
    FB = np.stack([np.cos(a), -np.sin(a), np.sin(a)], axis=1).astype(bf)
    GC = np.stack([np.concatenate([np.cos(a), np.sin(a)], axis=1), np.concatenate([-np.sin(a), np.cos(a)], axis=1)], axis=1).astype(bf)
    a = 2 * np.pi * np.arange(N1)[:, None] * np.arange(NH1)[None, :] / N1
    GA = np.stack([np.cos(a), -np.sin(a)], axis=1).astype(bf)
    return dict(cmats=cm, zT=np.ascontiguousarray(z.T), win=win, FA=FA, TW=TW, FB=FB, GC=GC, TWT=TWT, GA=GA)


def prep_A(inp, l, j, hT_b, L):
    w_in = inp["w_in"][l]; b_in = inp["b_in"][l]
    o = np.cumsum([0, 512, 512, 1024, 1024, 16, 512, 512, 1024, 1024, 32, 3072])
    mq, mk, mv, mo, mg, gq, gk, gv, gg, glr, hy = o[:11]
    cols = np.concatenate([
        mq + j * 128 + np.arange(128), mk + j * 128 + np.arange(128), mv + j * 256 + np.arange(256), mo + j * 256 + np.arange(256),
        mg + np.array([0 * 4 + j, 1 * 4 + j, 2 * 4 + j, 3 * 4 + j]),
        gq + j * 128 + np.arange(128), gk + j * 128 + np.arange(128), gv + j * 256 + np.arange(256), gg + j * 256 + np.arange(256),
        glr + np.arange(32)])
    assert len(cols) == NTOK
    hcols = np.concatenate([hy + s * 1024 + j * 256 + np.arange(256) for s in range(3)])
    lc = hcols - hy
    cwv = inp["hyena_conv_w"][l][:, lc]
    sk = inp["hyena_skip"][l][:, j * 256:(j + 1) * 256]
    w3 = inp["hyena_ffn_w3"][l].reshape(64, 2, 2, 1024)[:, :, :, j * 256:(j + 1) * 256].reshape(64, 1024)
    d = dict(
        g0=np.ascontiguousarray(inp["norm_gains"][l, 0].reshape(32, 128).T),
        Wtok=np.ascontiguousarray(w_in[:, cols]), btok=np.ascontiguousarray(b_in[cols][None]),
        Why=np.ascontiguousarray(w_in[:, hcols]), bhy=np.ascontiguousarray(b_in[hcols].reshape(6, 128).T),
        hn=np.concatenate([inp["mlstm_head_norm"][l][j * 256:(j + 1) * 256], inp["gla_head_norm"][l][j * 256:(j + 1) * 256]])[None].astype(np.float32),
        dU=np.ascontiguousarray(np.concatenate([inp["gla_decay_up"][l][0][:, j * 128:(j + 1) * 128], inp["gla_decay_up"][l][1][:, j * 128:(j + 1) * 128]], axis=1)),
        dB=np.concatenate([inp["gla_decay_bias"][l][0][j * 128:(j + 1) * 128], inp["gla_decay_bias"][l][1][j * 128:(j + 1) * 128]])[None].astype(np.float32),
        cw=np.ascontiguousarray(cwv.T.reshape(6, 128, 3).transpose(1, 0, 2)), cb=np.ascontiguousarray(inp["hyena_conv_b"][l][lc].reshape(6, 128).T),
        fw1=inp["hyena_ffn_w1"][l], fb1=inp["hyena_ffn_b1"][l][:, None], fw2=inp["hyena_ffn_w2"][l], fb2=inp["hyena_ffn_b2"][l][:, None],
        fw3=np.ascontiguousarray(w3), ffreq=inp["hyena_freq"][l][:, None],
        skipc=np.ascontiguousarray(sk.reshape(2, 2, 128).transpose(2, 0, 1).reshape(128, 4)))
    if hT_b is not None:
        d['hT'] = hT_b
    return {k: np.ascontiguousarray(np.asarray(v)) for k, v in d.items()}


UID = [0]


def uid():
    UID[0] += 1
    return UID[0]


def B_phase(ctx, C, W, hT, yT, memT, T, out, last, gt, bgt, xn_pre=None, M=256, DFF=4 * D):
    nc = ctx.nc
    u = uid()
    def gain(i):
        return col(gt, gt.ap[:, i * 32:(i + 1) * 32])
    dr = lambda shape, dt, name: ctx.dram(shape, dt, "%s_%d" % (name, u))
    xn = dr([D, T], BF16, "xn"); G = [dr([D, T], F32, "G%d" % n) for n in range(3)]; mg = dr([D, T], BF16, "mg")
    h1 = dr([D, T], F32, "h1"); qd = dr([D, T], BF16, "qd"); memn = dr([D, M], BF16, "memn")
    kTd = dr([D, M], BF16, "kTd"); vtok = dr([M, D], BF16, "vtok"); od = dr([D, T], BF16, "od")
    h2 = dr([D, T], F32, "h2"); hid = [dr([D, T], BF16, "hid%d" % g) for g in range(DFF // D)]
    hp = [dr([D, T], F32, "hp%d" % i) for i in range(3)]
    h3 = out if not last else dr([D, T], F32, "h3")
    if xn_pre is None:
        norm_fm(ctx, hT, gain(0), xn, T, C)
        xn0 = xn
    else:
        xn0 = xn_pre
    for n in range(3):
        def epi_gate(ctx, pl, o, nb, nn, t0, s, SW, n=n):
            ctx.op("act", lambda: nc.scalar.activation(out=o.ap[:nn, s * SW:(s + 1) * SW], in_=pl[0].ap[:nn, :SW], func=AF.Sigmoid, bias=bgt.ap[:nn, n * 32 + nb:n * 32 + nb + 1]),
                   outs=[o], ins=[pl[0], bgt], part=(s > 0))
        gemm_fm(ctx, [(W["Wg"][:, n * D:(n + 1) * D], xn0, 0, D)], G[n], D, T, epi_gate)
    with contextlib.ExitStack() as es2:
        gts = Rot([ctx.sb([128, 512], F32, "gt", es2) for _ in range(6)])
        tms = Rot([ctx.sb([128, 512], F32, "tm", es2) for _ in range(6)])
        def epi_merge(ctx, pl, o, nb, nn, t0, s, SW):
            ts_ = []
            for n in range(3):
                g = gts.next(); t = tms.next()
                ctx.dma(("sp", "pool")[n % 2], g.ap[:, :SW], G[n].ap[nb * 128:(nb + 1) * 128, t0:t0 + SW], outs=[g], ins=[G[n]])
                ctx.op("dve", lambda: nc.vector.tensor_tensor(out=t.ap[:, :SW], in0=pl[n].ap[:, :SW], in1=g.ap[:, :SW], op=ALU.mult), outs=[t], ins=[pl[n], g])
                ts_.append(t)
            ctx.op("pool", lambda: nc.gpsimd.tensor_tensor(out=ts_[0].ap[:, :SW], in0=ts_[0].ap[:, :SW], in1=ts_[1].ap[:, :SW], op=ALU.add), outs=[ts_[0]], ins=[ts_[0], ts_[1]])
            ctx.op("pool", lambda: nc.gpsimd.tensor_tensor(out=o.ap[:, s * SW:(s + 1) * SW], in0=ts_[0].ap[:, :SW], in1=ts_[2].ap[:, :SW], op=ALU.add), outs=[o], ins=[ts_[0], ts_[2]], part=(s > 0))
        gemm_fm(ctx, [(W["Wbr"][n], yT, n * 1024, 1024) for n in range(3)], mg, D, T, epi_merge)
    def make_epi_add(A, es_):
        ads = Rot([ctx.sb([128, 512], F32, "ad", es_) for _ in range(3)])
        def epi(ctx, pl, o, nb, nn, t0, s, SW):
            a = ads.next()
            ctx.dma(("sp", "pool")[nb % 2], a.ap[:, :SW], A.ap[nb * 128:(nb + 1) * 128, t0:t0 + SW], outs=[a], ins=[A])
            ctx.op("dve", lambda: nc.vector.tensor_tensor(out=o.ap[:, s * SW:(s + 1) * SW], in0=pl[0].ap[:, :SW], in1=a.ap[:, :SW], op=ALU.add),
                   outs=[o], ins=[pl[0], a], part=(s > 0))
        return epi
    def epi_copy(ctx, pl, o, nb, nn, t0, s, SW):
        ctx.op("act", lambda: nc.scalar.copy(out=o.ap[:nn, s * SW:(s + 1) * SW], in_=pl[0].ap[:nn, :SW]), outs=[o], ins=[pl[0]], part=(s > 0))
    with contextlib.ExitStack() as es2:
        gemm_fm(ctx, [(W["Wout"], mg, 0, D)], h1, D, T, make_epi_add(hT, es2))
    norm_fm(ctx, h1, gain(1), xn, T, C)
    gemm_fm(ctx, [(W["Wq"], xn, 0, D)], qd, D, T, epi_copy)
    norm_fm(ctx, memT, gain(2), memn, M, C)
    gemm_fm(ctx, [(W["Wkv"][:, 0:D], memn, 0, D)], kTd, D, M, epi_copy)
    gemm_tm(ctx, W["Wkv"][:, D:2 * D], memn, vtok, D, D, M)
    attention(ctx, qd, kTd, vtok, od, T, C)
    with contextlib.ExitStack() as es2:
        gemm_fm(ctx, [(W["Wo"], od, 0, D)], h2, D, T, make_epi_add(h1, es2))
    norm_fm(ctx, h2, gain(3), xn, T, C)
    with contextlib.ExitStack() as es2:
        rts = Rot([ctx.sb([128, 512], F32, "rt", es2) for _ in range(3)])
        def epi_relu2(ctx, pl, o, nb, nn, t0, s, SW):
            r = rts.next()
            ctx.op("act", lambda: nc.scalar.activation(out=r.ap[:, :SW], in_=pl[0].ap[:, :SW], func=AF.Relu), outs=[r], ins=[pl[0]])
            ctx.op("pool", lambda: nc.gpsimd.tensor_tensor(out=o.ap[:, s * SW:(s + 1) * SW], in0=r.ap[:, :SW], in1=r.ap[:, :SW], op=ALU.mult), outs=[o], ins=[r], part=(s > 0))
        for g in range(DFF // D):
            gemm_fm(ctx, [(W["W1"][:, g * D:(g + 1) * D], xn, 0, D)], hid[g], D, T, epi_relu2)
    NG = DFF // D
    dst = [hp[i % 3] for i in range(NG - 1)] + [h3]
    for g in range(NG):
        with contextlib.ExitStack() as es2:
            gemm_fm(ctx, [(W["W2"][g * D:(g + 1) * D, :], hid[g], 0, D)], dst[g], D, T, make_epi_add(h2 if g == 0 else dst[g - 1], es2))
    if last:
        norm_fm(ctx, h3, gain(4), out, T, C)


def A_unit(ctx, C, hin_c, W, j, xn, YT, L, scr):
    nc = ctx.nc
    ctx.barrier()
    with contextlib.ExitStack() as es:
        def lc(ap, shape, dt, name, q="sp"):
            t = ctx.sb(shape, dt, name, es)
            ctx.dma(q, t.ap[:], ap, outs=[t], ins=[])
            return t
        N1 = 2 * L // 128
        btok_bc = lc(W["btok"][j].partition_broadcast(128), [128, NTOK], F32, "btok_bc", "pool")
        bhyt = lc(W["bhy"][j], [128, 6], F32, "bhy")
        C["hn"] = lc(W["hn"][j].partition_broadcast(128), [128, 512], F32, "hn_bc", "pool")
        dUt = lc(W["dU"][j], [16, 256], F32, "dU"); dB_bc = lc(W["dB"][j].partition_broadcast(128), [128, 256], F32, "dB_bc", "pool")
        hin = dict(hin_c)
        hin.update(cw=lc(W["cw"][j], [128, 6, 3], F32, "cw"), cb=lc(W["cb"][j], [128, 6], F32, "cb"),
                   fw3=lc(W["fw3"][j], [64, 1024], F32, "fw3"), skipc=lc(W["skipc"][j], [128, 4], F32, "skipc"),
                   win=Buf("win%d" % j, W["win"][j]))
        P, HY, U, FILT, KS, Z1, OFm, OFg = scr
        gemm_tm(ctx, W["Wtok"][j], xn, P, D, NTOK, L, bias_bc=btok_bc)
        def epi_bias(ctx, pl, o, nb, nn, t0, s, SW):
            ctx.op("act", lambda: nc.scalar.activation(out=o.ap[:nn, s * SW:(s + 1) * SW], in_=pl[0].ap[:nn, :SW], func=AF.Identity, bias=bhyt.ap[:nn, nb:nb + 1]),
                   outs=[o], ins=[pl[0], bhyt], part=(s > 0))
        gemm_fm(ctx, [(W["Why"][j], xn, 0, D)], HY, 768, L, epi_bias)
        for bwd in (False, True):
            scan(ctx, C, P, L, "ml", bwd, (C_MLQ, C_MLV, C_MLG, 4), OFm, None, C["hn"].ap[:, 0:256], yT=(YT, j * 256))
        for bwd in (False, True):
            scan(ctx, C, P, L, "gla", bwd, (C_GQ, C_GV, C_GLR, 32), OFg, None, C["hn"].ap[:, 256:512], gl=(dUt, dB_bc), yT=(YT, 1024 + j * 256))
        yhy = col(YT, YT.ap[2048 + j * 256:2048 + (j + 1) * 256, :])
        hyena(ctx, C, HY, U, FILT, KS, Z1, yhy, L, hin)
        ctx.barrier()


def build_fused(L, TQ, depth=2, M=256, dbg=()):
    N1 = 2 * L // 128
    NH1 = N1 // 2
    nc = bass.Bass("TRN2", target_bir_lowering=False)
    def din(name, shape, dt=F32):
        return nc.dram_tensor(name, list(shape), dt, kind="ExternalInput").ap()
    xT = Buf("xT", din("xT", [D, L])); memT = Buf("memT", din("memT", [D, M]))
    Wl = []
    for l in range(depth):
        p = "l%d_" % l
        Wl.append(dict(
            g0=din(p + "g0", [128, 32]), gains=din(p + "gains", [128, 160]),
            Wtok=din(p + "Wtok", [4, D, NTOK]), btok=din(p + "btok", [4, 1, NTOK]), Why=din(p + "Why", [4, D, 768]), bhy=din(p + "bhy", [4, 128, 6]),
            hn=din(p + "hn", [4, 1, 512]), dU=din(p + "dU", [4, 16, 256]), dB=din(p + "dB", [4, 1, 256]),
            cw=din(p + "cw", [4, 128, 6, 3]), cb=din(p + "cb", [4, 128, 6]),
            fw1=din(p + "fw1", [33, 64]), fb1=din(p + "fb1", [64, 1]), fw2=din(p + "fw2", [64, 64]), fb2=din(p + "fb2", [64, 1]),
            fw3=din(p + "fw3", [4, 64, 1024]), ffreq=din(p + "ffreq", [64, 1]), skipc=din(p + "skipc", [4, 128, 4]),
            Wg=din(p + "Wg", [D, 3 * D]), bg=din(p + "bg", [128, 96]), Wbr=din(p + "Wbr", [3, 1024, D]), Wout=din(p + "Wout", [D, D]),
            Wq=din(p + "Wq", [D, D]), Wkv=din(p + "Wkv", [D, 2 * D]), Wo=din(p + "Wo", [D, D]), W1=din(p + "W1", [D, 4 * D]), W2=din(p + "W2", [4 * D, D])))
    cmats = din("cmats", [128, 5, 128]); zT = Buf("zT", din("zT", [33, L])); win = din("win", [4, 256, L])
    FA = din("FA", [NH1, 2 * N1], BF16); TW = din("TW", [128, 2, N1]); FB = din("FB", [128, 3, 128], BF16)
    GC = din("GC", [128, 2, 256], BF16); TWT = din("TWT", [N1, 2, 128]); GA = din("GA", [N1, 2, NH1], BF16)
    out = Buf("out", nc.dram_tensor("out", [D, TQ], F32, kind="ExternalOutput").ap())
    with contextlib.ExitStack() as es:
        ctx = Ctx(nc, es)
        ctx.dbg = dbg
        pid = nc.sync.partition_id()
        C = make_consts(ctx, None)
        cm = load_const(ctx, cmats, [128, 5, 128], F32, "cmats")
        for i, n in enumerate(["ident", "UT", "LT", "SLo", "SUp"]):
            C[n] = col(cm, cm.ap[:, i, :])
        C["ident_bf"] = ctx.sb([128, 128], BF16, "ident_bf")
        ctx.op("dve", lambda: nc.vector.tensor_copy(out=C["ident_bf"].ap[:], in_=C["ident"].ap), outs=[C["ident_bf"]], ins=[cm])
        C["zeros"] = ctx.sb([128, 128], F32, "zeros")
        ctx.op("pool", lambda: nc.gpsimd.memset(C["zeros"].ap[:], 0.0), outs=[C["zeros"]], ins=[])
        C["one_col"] = ctx.sb([128, 1], F32, "one_col")
        ctx.op("pool", lambda: nc.gpsimd.memset(C["one_col"].ap[:], 1.0), outs=[C["one_col"]], ins=[])
        hc = dict(zT=zT, FA=load_const(ctx, FA, [NH1, 2 * N1], BF16, "FA"))
        twt = load_const(ctx, TW, [128, 2, N1], F32, "TW"); hc["TWr"] = col(twt, twt.ap[:, 0, :]); hc["TWi"] = col(twt, twt.ap[:, 1, :])
        fbt = load_const(ctx, FB, [128, 3, 128], BF16, "FB")
        hc["FBr"] = col(fbt, fbt.ap[:, 0, :]); hc["FBi"] = col(fbt, fbt.ap[:, 1, :]); hc["FBin"] = col(fbt, fbt.ap[:, 2, :])
        gct = load_const(ctx, GC, [128, 2, 256], BF16, "GC"); hc["GC1"] = col(gct, gct.ap[:, 0, :]); hc["GC2"] = col(gct, gct.ap[:, 1, :])
        twtt = load_const(ctx, TWT, [N1, 2, 128], F32, "TWT"); hc["TWTr"] = col(twtt, twtt.ap[:, 0, :]); hc["TWTi"] = col(twtt, twtt.ap[:, 1, :])
        gat = load_const(ctx, GA, [N1, 2, NH1], BF16, "GA"); hc["GAr"] = col(gat, gat.ap[:, 0, :]); hc["GAin"] = col(gat, gat.ap[:, 1, :])
        scr = (ctx.dram([L, NTOK], F32, "P"), ctx.dram([768, L], F32, "HY"), ctx.dram([768, L], F32, "U"), ctx.dram([1024, L], BF16, "FILT"),
               ctx.dram([2, 2, 128, 256, N1], F32, "KS"), ctx.dram([256, L], F32, "Z1"), ctx.dram([L, 256], F32, "OFm"), ctx.dram([L, 256], F32, "OFg"))
        xnA = ctx.dram([D, L], BF16, "xnA"); YT = ctx.dram([3 * 1024, L], BF16, "YT")
        hfull = xT
        for l in range(depth):
            W = Wl[l]
            last = l == depth - 1
            with contextlib.ExitStack() as esl:
                g0t = ctx.sb([128, 32], F32, "g0", esl); ctx.dma("sp", g0t.ap[:], W["g0"], outs=[g0t], ins=[])
                gt = ctx.sb([128, 160], F32, "gains", esl); ctx.dma("sp", gt.ap[:], W["gains"], outs=[gt], ins=[])
                bgt = ctx.sb([128, 96], F32, "bg", esl); ctx.dma("pool", bgt.ap[:], W["bg"], outs=[bgt], ins=[])
                hl = dict(hc)
                hl["fw1"] = ctx.sb([33, 64], F32, "fw1", esl); ctx.dma("sp", hl["fw1"].ap[:], W["fw1"], outs=[hl["fw1"]], ins=[])
                hl["fw2"] = ctx.sb([64, 64], F32, "fw2", esl); ctx.dma("sp", hl["fw2"].ap[:], W["fw2"], outs=[hl["fw2"]], ins=[])
                hl["ffreq"] = ctx.sb([64, 1], F32, "ffreq", esl); ctx.dma("sp", hl["ffreq"].ap[:], W["ffreq"], outs=[hl["ffreq"]], ins=[])
                b1t = ctx.sb([64, 1], F32, "fb1", esl); ctx.dma("sp", b1t.ap[:], W["fb1"], outs=[b1t], ins=[])
                b2t = ctx.sb([64, 1], F32, "fb2", esl); ctx.dma("sp", b2t.ap[:], W["fb2"], outs=[b2t], ins=[])
                hl["ffb1"] = ctx.sb([64, 1], F32, "ffb1", esl); hl["ffb2"] = ctx.sb([64, 1], F32, "ffb2", esl)
                ctx.op("dve", lambda: nc.vector.tensor_tensor(out=hl["ffb1"].ap[:], in0=b1t.ap[:], in1=hl["ffreq"].ap[:], op=ALU.mult), outs=[hl["ffb1"]], ins=[b1t, hl["ffreq"]])
                ctx.op("dve", lambda: nc.vector.tensor_tensor(out=hl["ffb2"].ap[:], in0=b2t.ap[:], in1=hl["ffreq"].ap[:], op=ALU.mult), outs=[hl["ffb2"]], ins=[b2t, hl["ffreq"]])
                norm_fm(ctx, hfull, g0t, xnA, L, C)
                Wj = dict(W); Wj["win"] = win
                for j in range(4):
                    A_unit(ctx, C, hl, Wj, j, xnA, YT, L, scr)
                if not last:
                    hnext = ctx.dram([D, L], F32, "hfull%d" % l)
                    B_phase(ctx, C, W, hfull, YT, memT, L, hnext, False, gt, bgt, xn_pre=xnA, M=M)
                    hfull = hnext
                else:
                    hd = [ctx.dram([D // 2, 2 * L], F32, "hdup%d" % r) for r in range(2)]; yd = ctx.dram([3 * 1024, 2 * L], BF16, "ydup")
                    hq = ctx.dram([D, TQ], F32, "hq"); yq = ctx.dram([3 * 1024, TQ], BF16, "yq")
                    ctx.barrier()
                    for hh in range(2):
                        for r in range(2):
                            ctx.dma("sp", hd[r].ap[:, hh * L:(hh + 1) * L], hfull.ap[r * (D // 2):(r + 1) * (D // 2), :], outs=[hd[r]], ins=[hfull], part=True)
                        ctx.dma("sp", yd.ap[:, hh * L:(hh + 1) * L], YT.ap[:, :], outs=[yd], ins=[YT], part=True)
                    for r in range(2):
                        ctx.dma("sp", hq.ap[r * (D // 2):(r + 1) * (D // 2), :], hd[r].ap[:, bass.ds(pid * TQ, TQ)], outs=[hq], ins=[hd[r]], part=True)
                    ctx.dma("sp", yq.ap[:, :], yd.ap[:, bass.ds(pid * TQ, TQ)], outs=[yq], ins=[yd])
                    B_phase(ctx, C, W, hq, yq, memT, TQ, out, True, gt, bgt, M=M)
                ctx.barrier()
        ctx.finish([out])
    return nc


def prep_fused(inp, b, L, cA):
    d = dict(xT=np.ascontiguousarray(inp["x"][b].T), memT=np.ascontiguousarray(inp["mem"][b].T))
    depth = inp["w_in"].shape[0]
    for l in range(depth):
        p = "l%d_" % l
        pj = [prep_A(inp, l, j, None, L) for j in range(4)]
        for k in ("Wtok", "btok", "Why", "bhy", "hn", "dU", "dB", "cw", "cb", "fw3", "skipc"):
            d[p + k] = np.ascontiguousarray(np.stack([pj[j][k] for j in range(4)]))
        for k in ("g0", "fw1", "fb1", "fw2", "fb2", "ffreq"):
            d[p + k] = pj[0][k]
        ng = inp["norm_gains"][l]
        d[p + "gains"] = np.ascontiguousarray(np.stack([ng[0], ng[1], ng[2], ng[3], inp["final_norm"]]).reshape(5, 32, 128).transpose(2, 0, 1).reshape(128, 160))
        G0 = inp["w_in"].shape[2] - 3 * D
        d[p + "Wg"] = np.ascontiguousarray(inp["w_in"][l][:, G0:]); d[p + "bg"] = np.ascontiguousarray(inp["b_in"][l][G0:].reshape(96, 128).T)
        d[p + "Wbr"] = inp["w_branch"][l]; d[p + "Wout"] = inp["w_out"][l]; d[p + "Wq"] = inp["xattn_wq"][l]; d[p + "Wkv"] = inp["xattn_wkv"][l]
        d[p + "Wo"] = inp["xattn_wo"][l]; d[p + "W1"] = inp["mlp_w1"][l]; d[p + "W2"] = inp["mlp_w2"][l]
    for k in ("cmats", "zT", "FA", "TW", "FB", "GC", "TWT", "GA"):
        d[k] = cA[0][k]
    d["win"] = np.ascontiguousarray(np.stack([cA[j]["win"] for j in range(4)]))
    return d


N_CORES = 8
SEQ = 8192
TOK_B = 2048
GATE0 = 21552 - 3 * D

_CACHE = {}


def _prog(key, fn):
    if key not in _CACHE:
        _CACHE[key] = fn()
    return _CACHE[key]


def prep_B(inp, l, hT_sl, yT_sl, memT_b):
    ng = inp["norm_gains"][l]
    gains = np.stack([ng[0], ng[1], ng[2], ng[3], inp["final_norm"]]).reshape(5, 32, 128).transpose(2, 0, 1).reshape(128, 160)
    return dict(hT=np.ascontiguousarray(hT_sl), yT=np.ascontiguousarray(yT_sl), memT=memT_b, gains=np.ascontiguousarray(gains),
                Wg=inp["_Wg"][l], bg=np.ascontiguousarray(inp["b_in"][l][GATE0:].reshape(96, 128).T),
                Wbr=inp["w_branch"][l], Wout=inp["w_out"][l], Wq=inp["xattn_wq"][l], Wkv=inp["xattn_wkv"][l], Wo=inp["xattn_wo"][l],
                W1=inp["mlp_w1"][l], W2=inp["mlp_w2"][l])


def kernel(**inputs):
    inp = {k: np.asarray(v) for k, v in inputs.items()}
    x = inp["x"]; mem = inp["mem"]
    NB_, L, _ = x.shape
    depth = inp["w_in"].shape[0]
    inp["_Wg"] = [np.ascontiguousarray(inp["w_in"][l][:, GATE0:]) for l in range(depth)]
    ncA = _prog(("A", L), lambda: build_A(L))
    cA = [consts_A(L, j) for j in range(4)]
    hT = [np.ascontiguousarray(x[b].T) for b in range(NB_)]
    memT = [np.ascontiguousarray(mem[b].T) for b in range(NB_)]
    cores = list(range(N_CORES))
    for l in range(depth):
        maps = []
        for c in cores:
            b, j = divmod(c, 4)
            m = prep_A(inp, l, j, hT[b], L)
            m.update(cA[j])
            maps.append(m)
        resA = run_bass_kernel_spmd(ncA, maps, core_ids=cores).results
        del maps
        yT = []
        for b in range(NB_):
            ml = np.concatenate([resA[b * 4 + j]["y_ml"] for j in range(4)], axis=1)
            gl = np.concatenate([resA[b * 4 + j]["y_gla"] for j in range(4)], axis=1)
            hy = np.concatenate([resA[b * 4 + j]["y_hy"] for j in range(4)], axis=0)
            yT.append(np.concatenate([ml.T, gl.T, hy], axis=0))
        last = l == depth - 1
        ncB = _prog(("B", last), lambda: build_B(TOK_B, last))
        mapsB = []
        for c in cores:
            b, q = divmod(c, 4)
            sl = slice(q * TOK_B, (q + 1) * TOK_B)
            mapsB.append(prep_B(inp, l, hT[b][:, sl], yT[b][:, sl], memT[b]))
        resB = run_bass_kernel_spmd(ncB, mapsB, core_ids=cores).results
        del mapsB
        hT = [np.concatenate([resB[b * 4 + q]["out"] for q in range(4)], axis=1) for b in range(NB_)]
    out = np.stack([np.ascontiguousarray(hT[b].T) for b in range(NB_)]).astype(np.float32)
    return out
```
